# Optimizing a Trainium2 kernel written in Bass

```python
import jax, jax.numpy as jnp
from jax import lax
import numpy as np

D_MODEL = 1024
BATCH = 16
SEQ = 4096
DEPTH = 1

POOL_WIDTH = D_MODEL // 2
POOL_GROUPS = 4
POOL_GROUP_DIM = POOL_WIDTH // POOL_GROUPS
POOL_WINDOWS = (2, 4, 8, 16)
LRU_WIDTH = D_MODEL
LRU_HEADS = 16
LRU_HEAD_DIM = LRU_WIDTH // LRU_HEADS
CONV_WIDTH = 4
RG_C = 8.0
IN_COLS = POOL_WIDTH + 2 * LRU_WIDTH + 2 * D_MODEL
N_GROUPS = 4
EXP_PER_GROUP = 4
N_EXP = N_GROUPS * EXP_PER_GROUP
TOP_K = 2
D_EXPERT = D_MODEL // 2
EPS = 1e-6

kernel_name = 'hybrid_pool_rglru_hmoe_block'


def rms_norm(x, g):
    xf = x.astype(jnp.float32)
    y = xf * lax.rsqrt(jnp.mean(xf * xf, axis=-1, keepdims=True) + EPS)
    return (y * g.astype(jnp.float32)).astype(x.dtype)


def pooling_mixer(u, pool_w, pool_scale):
    b_, s_, _ = u.shape
    ug = u.astype(jnp.float32).reshape(b_, s_, POOL_GROUPS, POOL_GROUP_DIM)
    c = jnp.cumsum(ug, axis=1)
    pos = jnp.arange(1, s_ + 1, dtype=jnp.float32)
    pooled = []
    for g, w in enumerate(POOL_WINDOWS):
        cg = c[:, :, g]
        prev = jnp.pad(cg[:, :s_ - w], ((0, 0), (w, 0), (0, 0)))
        cnt = jnp.minimum(pos, float(w))
        pooled.append((cg - prev) / cnt[None, :, None])
    z = (jnp.stack(pooled, axis=2) - ug).astype(u.dtype)
    y = jnp.einsum('bsgc,gcd->bsgd', z, pool_w).reshape(b_, s_, POOL_WIDTH)
    return y * pool_scale


def rglru_mixer(u, gate_in, conv_w, conv_b, w_r, b_r, w_i, b_i, lam):
    b_, s_, c_ = u.shape
    xc = lax.conv_general_dilated(
        u, conv_w[:, None, :], window_strides=(1,), padding=[(CONV_WIDTH - 1, 0)],
        dimension_numbers=('NWC', 'WIO', 'NWC'), feature_group_count=c_) + conv_b
    xh = xc.reshape(b_, s_, LRU_HEADS, LRU_HEAD_DIM)
    r = jax.nn.sigmoid(jnp.einsum('bshc,hcd->bshd', xh, w_r).reshape(b_, s_, c_) + b_r)
    i = jax.nn.sigmoid(jnp.einsum('bshc,hcd->bshd', xh, w_i).reshape(b_, s_, c_) + b_i)
    log_a = -RG_C * r.astype(jnp.float32) * jax.nn.softplus(-lam.astype(jnp.float32))
    a = jnp.exp(log_a)
    mult = jnp.sqrt(-jnp.expm1(2.0 * log_a))
    bt = mult * (i * xc).astype(jnp.float32)

    def combine(lhs, rhs):
        a1, b1 = lhs
        a2, b2 = rhs
        return a1 * a2, a2 * b1 + b2

    _, h = lax.associative_scan(combine, (a, bt), axis=1)
    return h.astype(u.dtype) * jax.nn.gelu(gate_in)


def hier_moe(h, rg_w, rg_b, re_w, re_b, e_gate, e_up, e_down):
    b_, s_, d_ = h.shape
    xt = h.reshape(b_ * s_, d_)
    g_logits = (xt @ rg_w + rg_b).astype(jnp.float32)
    g_prob = jax.nn.softmax(g_logits, axis=-1)
    p_top, g_idx = lax.top_k(g_prob, 1)
    e_logits = (xt @ re_w + re_b).astype(jnp.float32).reshape(-1, N_GROUPS, EXP_PER_GROUP)
    sel = jnp.take_along_axis(e_logits, g_idx[:, :, None], axis=1)[:, 0]
    v, e_idx = lax.top_k(sel, TOP_K)
    w2 = jax.nn.softmax(v, axis=-1) * p_top
    ids = g_idx * EXP_PER_GROUP + e_idx
    comb = jnp.einsum('tk,tke->te', w2, jax.nn.one_hot(ids, N_EXP, dtype=jnp.float32)).astype(h.dtype)
    y = jnp.zeros_like(xt)
    for e in range(N_EXP):
        he = jax.nn.silu(xt @ e_gate[e]) * (xt @ e_up[e])
        y = y + comb[:, e:e + 1] * (he @ e_down[e])
    return y.reshape(b_, s_, d_)


def setup_inputs(seed: int = 0) -> dict:
    key = jax.random.key(seed)
    ks = jax.random.split(key, 24)
    f32 = jnp.float32
    L = DEPTH

    def nrm(k, shape, scale):
        return jax.random.normal(k, shape, f32) * scale

    u = jax.random.uniform(ks[10], (L, LRU_WIDTH), f32, 0.9, 0.999)
    s = u ** (1.0 / RG_C)
    lam = jnp.log(s) - jnp.log1p(-s)
    return {
        'x': nrm(ks[0], (BATCH, SEQ, D_MODEL), 1.0),
        'norm1_g': 1.0 + nrm(ks[1], (L, D_MODEL), 0.02),
        'w_in': nrm(ks[2], (L, D_MODEL, IN_COLS), D_MODEL ** -0.5),
        'pool_w': nrm(ks[3], (L, POOL_GROUPS, POOL_GROUP_DIM, POOL_GROUP_DIM), POOL_GROUP_DIM ** -0.5),
        'pool_scale': 1.0 + nrm(ks[4], (L, POOL_WIDTH), 0.02),
        'conv_w': nrm(ks[5], (L, CONV_WIDTH, LRU_WIDTH), CONV_WIDTH ** -0.5),
        'conv_b': nrm(ks[6], (L, LRU_WIDTH), 0.01),
        'rg_w_r': nrm(ks[7], (L, LRU_HEADS, LRU_HEAD_DIM, LRU_HEAD_DIM), LRU_HEAD_DIM ** -0.5),
        'rg_b_r': nrm(ks[8], (L, LRU_WIDTH), 0.01),
        'rg_w_i': nrm(ks[9], (L, LRU_HEADS, LRU_HEAD_DIM, LRU_HEAD_DIM), LRU_HEAD_DIM ** -0.5),
        'rg_b_i': nrm(ks[11], (L, LRU_WIDTH), 0.01),
        'rg_lambda': lam,
        'proj_a': nrm(ks[12], (L, POOL_WIDTH, D_MODEL), POOL_WIDTH ** -0.5),
        'proj_b': nrm(ks[13], (L, LRU_WIDTH, D_MODEL), LRU_WIDTH ** -0.5),
        'w_out': nrm(ks[14], (L, D_MODEL, D_MODEL), D_MODEL ** -0.5),
        'norm2_g': 1.0 + nrm(ks[15], (L, D_MODEL), 0.02),
        'router_group_w': nrm(ks[16], (L, D_MODEL, N_GROUPS), D_MODEL ** -0.5),
        'router_group_b': nrm(ks[17], (L, N_GROUPS), 0.01),
        'router_expert_w': nrm(ks[18], (L, D_MODEL, N_EXP), D_MODEL ** -0.5),
        'router_expert_b': nrm(ks[19], (L, N_EXP), 0.01),
        'exp_w_gate': nrm(ks[20], (L, N_EXP, D_MODEL, D_EXPERT), D_MODEL ** -0.5),
        'exp_w_up': nrm(ks[21], (L, N_EXP, D_MODEL, D_EXPERT), D_MODEL ** -0.5),
        'exp_w_down': nrm(ks[22], (L, N_EXP, D_EXPERT, D_MODEL), D_EXPERT ** -0.5),
        'norm_f_g': 1.0 + nrm(ks[23], (D_MODEL,), 0.02),
    }


def reference(x, norm1_g, w_in, pool_w, pool_scale, conv_w, conv_b, rg_w_r, rg_b_r, rg_w_i, rg_b_i,
              rg_lambda, proj_a, proj_b, w_out, norm2_g, router_group_w, router_group_b,
              router_expert_w, router_expert_b, exp_w_gate, exp_w_up, exp_w_down, norm_f_g):
    o1 = POOL_WIDTH
    o2 = o1 + LRU_WIDTH
    o3 = o2 + LRU_WIDTH
    o4 = o3 + D_MODEL
    for l in range(DEPTH):
        h = rms_norm(x, norm1_g[l])
        z = h @ w_in[l]
        u_pool, u_lru, u_gate = z[..., :o1], z[..., o1:o2], z[..., o2:o3]
        g_a = jax.nn.sigmoid(z[..., o3:o4])
        g_b = jax.nn.sigmoid(z[..., o4:])
        y_a = pooling_mixer(u_pool, pool_w[l], pool_scale[l]) @ proj_a[l]
        y_b = rglru_mixer(u_lru, u_gate, conv_w[l], conv_b[l], rg_w_r[l], rg_b_r[l],
                          rg_w_i[l], rg_b_i[l], rg_lambda[l]) @ proj_b[l]
        x = x + (g_a * y_a + g_b * y_b) @ w_out[l]
        h2 = rms_norm(x, norm2_g[l])
        x = x + hier_moe(h2, router_group_w[l], router_group_b[l], router_expert_w[l],
                         router_expert_b[l], exp_w_gate[l], exp_w_up[l], exp_w_down[l])
    return rms_norm(x, norm_f_g)
```

```python
import os
from contextlib import ExitStack

import numpy as np
import concourse.bass as bass
import concourse.mybir as mybir
from concourse.bass_utils import run_bass_kernel_spmd

F32 = mybir.dt.float32
BF16 = mybir.dt.bfloat16
ALU = mybir.AluOpType
AF = mybir.ActivationFunctionType
AX = mybir.AxisListType

ENGS = ("pe", "act", "dve", "pool", "sp")

D = 1024
NCORES = 8
EPS = 1e-6


class Op:
    __slots__ = ("eng", "fn", "deps", "tag", "signal", "val", "waits")

    def __init__(self, eng, fn, tag):
        self.eng = eng
        self.fn = fn
        self.tag = tag
        self.deps = []
        self.signal = False
        self.val = None
        self.waits = []


class Sched:
    def __init__(self, nc):
        self.nc = nc
        self.q = {e: [] for e in ENGS}
        self.reg = {}
        self.nops = 0

    def add(self, eng, fn, reads=(), writes=(), tag=None):
        op = Op(eng, fn, tag)
        self.nops += 1
        deps = {}
        for r in reads:
            st = self.reg.get(r)
            if st is not None and st[0] is not None:
                deps[id(st[0])] = st[0]
        for w in writes:
            st = self.reg.get(w)
            if st is not None:
                if st[0] is not None:
                    deps[id(st[0])] = st[0]
                for o in st[1]:
                    deps[id(o)] = o
        for r in reads:
            st = self.reg.get(r)
            if st is None:
                st = [None, []]
                self.reg[r] = st
            st[1].append(op)
        for w in writes:
            self.reg[w] = [op, []]
        deps.pop(id(op), None)
        op.deps = list(deps.values())
        self.q[eng].append(op)
        return op

    def barrier(self):
        lasts = [self.q[e][-1] for e in ENGS if self.q[e]]
        last_dma = {}
        for e in ENGS:
            for o in self.q[e]:
                if o.tag is not None:
                    last_dma[o.tag] = o
        deps = lasts + list(last_dma.values())
        for e in ENGS:
            op = Op(e, None, None)
            self.nops += 1
            op.deps = [d for d in deps]
            self.q[e].append(op)
        self.reg = {}

    def emit(self, mksem):
        nc = self.nc
        for e in ENGS:
            for op in self.q[e]:
                for d in op.deps:
                    if d.tag is None and d.eng == op.eng and op.eng == "pe":
                        continue
                    if d.fn is None:
                        continue
                    d.signal = True
        cnt = {}
        for e in ENGS:
            for op in self.q[e]:
                if op.fn is None:
                    continue
                if op.tag is not None:
                    key = ("dma", op.tag)
                    cnt[key] = cnt.get(key, 0) + 16
                    op.val = (key, cnt[key])
                    op.signal = True
                elif op.signal:
                    key = ("eng", e)
                    cnt[key] = cnt.get(key, 0) + 1
                    op.val = (key, cnt[key])
        sems = {key: mksem("s_" + "_".join(str(k) for k in key)) for key in cnt}
        nwaits = 0
        for e in ENGS:
            known = {}
            for op in self.q[e]:
                need = {}
                for d in op.deps:
                    if d.fn is None:
                        continue
                    if d.tag is None and d.eng == op.eng and op.eng == "pe":
                        continue
                    key, v = d.val
                    if known.get(key, 0) >= v:
                        continue
                    if need.get(key, 0) < v:
                        need[key] = v
                for key, v in need.items():
                    known[key] = v
                op.waits = list(need.items())
                nwaits += len(op.waits)
        self.nwaits = nwaits
        engobj = {"pe": "tensor", "act": "scalar", "dve": "vector", "pool": "gpsimd", "sp": "sync"}
        with nc.Block() as block:
            for e in ENGS:
                ops = self.q[e]
                if not ops:
                    continue

                def body(eng, ops=ops):
                    for op in ops:
                        for key, v in op.waits:
                            eng.wait_ge(sems[key], v)
                        if op.fn is None:
                            continue
                        ins = op.fn(eng)
                        if op.signal:
                            key, v = op.val
                            ins.then_inc(sems[key], 16 if op.tag is not None else 1)

                getattr(block, engobj[e])(body)


class Arena:
    def __init__(self, tensor, n):
        self.t = tensor
        self.n = n
        self.off = 0

    def alloc(self, shape, dtype):
        size = 2 if dtype == BF16 else 4
        nel = int(np.prod(shape))
        nb16 = (nel * size + 1) // 2
        nb16 = (nb16 + 15) // 16 * 16
        if self.off + nb16 > self.n:
            raise RuntimeError(f"arena overflow: need {self.off + nb16} > {self.n}")
        ap = self.t[:, self.off:self.off + nb16]
        self.off += nb16
        if size == 4:
            ap = ap.bitcast(dtype)[:, 0:nel]
        else:
            ap = ap[:, 0:nel]
        if len(shape) == 2:
            ap = ap.rearrange("p (a b) -> p a b", a=shape[0])
        elif len(shape) == 3:
            ap = ap.rearrange("p (a b c) -> p a b c", a=shape[0], b=shape[1])
        return ap


def build_sparse_phase2(L):
    nc = L["nc"]; S = L["S"]; A = L["A"]; banks = L["banks"]
    T = L["T"]; NE = L["NE"]; NJ = L["NJ"]; NSLOT = L["NSLOT"]
    x1s = L["x1s"]; out = L["out"]; tokof = L["tokof"]; Ys = L["Ys"]; wall = L["wall"]
    ident = L["ident"]; ones_bf = L["ones_bf"]; rb_bf = L["rb_bf"]; rw = L["rw"]
    g2bc = L["g2bc"]; gfbc = L["gfbc"]; onesf = L["onesf"]
    I32 = mybir.dt.int32
    T2 = 512
    NT2 = T // T2
    NST = T // 128
    WROW = 12288

    S.barrier()
    A.off = L["A1_start"]
    EQ1 = A.alloc([NST, 16], F32)
    EQ2 = A.alloc([NST, 16], F32)
    W1 = A.alloc([NST], F32)
    W2 = A.alloc([NST], F32)
    pos1i = A.alloc([NST], I32)
    pos2i = A.alloc([NST], I32)
    widx = A.alloc([NJ], I32)
    x1t = [A.alloc([4, D], F32) for _ in range(3)]
    junk2 = A.alloc([D], BF16)
    hn2 = [A.alloc([D], BF16) for _ in range(4)]
    hn4 = hn2
    ssq2 = A.alloc([4], F32)
    std2 = A.alloc([4], F32)
    rstd2 = A.alloc([4], F32)
    h2T = A.alloc([8, T2], BF16)
    h2Ts = [h2T, A.alloc([8, T2], BF16)]
    lg = A.alloc([4, 20], F32)
    gmax = A.alloc([4], F32)
    gsh = A.alloc([4, 4], F32)
    gsum = A.alloc([4], F32)
    ptop = A.alloc([4], F32)
    gmask = A.alloc([4, 4], F32)
    elm = A.alloc([4, 16], F32)
    mx8 = A.alloc([4, 8], F32)
    dd = A.alloc([4], F32)
    off_2b = A.off

    def norm_transpose(X, xnames):
        for s in range(4):
            S.add("act", lambda e, s=s: e.activation(out=junk2, in_=X[:, s, :], func=AF.Square, accum_out=ssq2[:, s:s + 1]),
                  reads=list(xnames), writes=["ssq2"])
        S.add("act", lambda e: e.activation(out=std2, in_=ssq2, func=AF.Sqrt, scale=1.0 / D, bias=EPS),
              reads=["ssq2"], writes=["std2"])
        S.add("dve", lambda e: e.reciprocal(out=rstd2, in_=std2), reads=["std2"], writes=["rstd2"])
        for s in range(4):
            hb = s % 2
            S.add("dve", lambda e, s=s, hb=hb: e.tensor_scalar(out=hn2[hb], in0=X[:, s, :], scalar1=rstd2[:, s:s + 1],
                                                              scalar2=None, op0=ALU.mult),
                  reads=list(xnames) + ["rstd2"], writes=[f"hn2_{hb}"])
            tb = s % 2
            tp = banks[tb][:].bitcast(BF16).rearrange("p (a b) -> p a b", a=8)
            for kc in range(8):
                S.add("pe", lambda e, kc=kc, hb=hb, tp=tp: e.transpose(out=tp[:, kc, :], in_=hn2[hb][:, kc * 128:(kc + 1) * 128],
                                                                       identity=ident),
                      reads=[f"hn2_{hb}", "ident"], writes=[f"bank{tb}"])
            S.add("dve", lambda e, tp=tp, s=s: e.tensor_tensor(out=h2T[:, :, s * 128:(s + 1) * 128], in0=tp, in1=g2bc, op=ALU.mult),
                  reads=[f"bank{tb}", "g2bc"], writes=["h2T"])

    off_2a_tmp = A.off
    LG = A.alloc([NST, 20], F32)
    gmaxA = A.alloc([NST], F32)
    gshA = A.alloc([NST, 4], F32)
    gsumA = A.alloc([NST], F32)
    ptopA = A.alloc([NST], F32)
    gmaskA = A.alloc([NST, 4], F32)
    elmA = A.alloc([NST, 16], F32)
    mx8A = A.alloc([NST, 8], F32)
    ddA = A.alloc([NST], F32)
    def a_load(ti):
        xb = ti % 3
        S.add("sp", lambda e, xb=xb, ti=ti: e.dma_start(out=x1t[xb], in_=x1s[ti * T2:(ti + 1) * T2, :].rearrange("(s p) d -> p s d", p=128)),
              writes=[f"x1t{xb}"], tag=f"x1t{xb}")

    def a_norm(ti):
        xb = ti % 3
        X = x1t[xb]
        for s in range(4):
            S.add("act", lambda e, s=s, X=X: e.activation(out=junk2, in_=X[:, s, :], func=AF.Square, accum_out=ssq2[:, s:s + 1]),
                  reads=[f"x1t{xb}"], writes=["ssq2"])
        S.add("act", lambda e: e.activation(out=std2, in_=ssq2, func=AF.Sqrt, scale=1.0 / D, bias=EPS),
              reads=["ssq2"], writes=["std2"])
        S.add("dve", lambda e: e.reciprocal(out=rstd2, in_=std2), reads=["std2"], writes=["rstd2"])
        for s in range(4):
            S.add("dve", lambda e, s=s, X=X: e.tensor_scalar(out=hn4[s], in0=X[:, s, :], scalar1=rstd2[:, s:s + 1],
                                                            scalar2=None, op0=ALU.mult),
                  reads=[f"x1t{xb}", "rstd2"], writes=[f"hn4_{s}"])

    def a_T(ti):
        hb = ti % 2
        for s in range(4):
            tb = s % 2
            tp = banks[tb][:].bitcast(BF16).rearrange("p (a b) -> p a b", a=8)
            for kc in range(8):
                S.add("pe", lambda e, kc=kc, s=s, tp=tp: e.transpose(out=tp[:, kc, :], in_=hn4[s][:, kc * 128:(kc + 1) * 128], identity=ident),
                      reads=[f"hn4_{s}"], writes=[f"bank{tb}"])
            S.add("dve", lambda e, tp=tp, s=s, hb=hb: e.tensor_tensor(out=h2Ts[hb][:, :, s * 128:(s + 1) * 128], in0=tp, in1=g2bc, op=ALU.mult),
                  reads=[f"bank{tb}"], writes=[f"h2T{hb}"])

    def a_logits(ti):
        hb = ti % 2
        lb = 2 + ti % 2
        lgp = banks[lb][:, 0:80].rearrange("p (s n) -> p s n", s=4)
        for s in range(4):
            for kc in range(8):
                S.add("pe", lambda e, s=s, kc=kc, lgp=lgp, hb=hb: e.matmul(lgp[:, s, :], lhsT=h2Ts[hb][:, kc, s * 128:(s + 1) * 128], rhs=rw[:, kc, :],
                                                                           start=(kc == 0), stop=False),
                      reads=[f"h2T{hb}"], writes=[f"bank{lb}"])
            S.add("pe", lambda e, s=s, lgp=lgp: e.matmul(lgp[:, s, :], lhsT=ones_bf[0:1, :], rhs=rb_bf[0:1, 0:20], start=False, stop=True),
                  reads=[], writes=[f"bank{lb}"])
        S.add("act", lambda e, lgp=lgp, ti=ti: e.activation(out=LG[:, ti * 4:(ti + 1) * 4, :], in_=lgp, func=AF.Copy),
              reads=[f"bank{lb}"], writes=["LG"])

    a_load(0)
    if NT2 > 1:
        a_load(1)
    a_norm(0)
    a_T(0)
    for ti in range(NT2):
        if ti + 2 < NT2:
            a_load(ti + 2)
        if ti + 1 < NT2:
            a_norm(ti + 1)
        a_logits(ti)
        if ti + 1 < NT2:
            a_T(ti + 1)
    GL = LG[:, :, 0:4]
    EL = LG[:, :, 4:20]
    S.add("dve", lambda e: e.tensor_reduce(out=gmaxA, in_=GL, axis=AX.X, op=ALU.max), reads=["LG"], writes=["gmax"])
    gmax_bc = gmaxA.unsqueeze(2).to_broadcast([128, NST, 4])
    S.add("dve", lambda e: e.tensor_tensor(out=gshA, in0=GL, in1=gmax_bc, op=ALU.subtract), reads=["LG", "gmax"], writes=["gsh"])
    S.add("dve", lambda e: e.tensor_tensor(out=gmaskA, in0=GL, in1=gmax_bc, op=ALU.is_equal), reads=["LG", "gmax"], writes=["gmask"])
    S.add("act", lambda e: e.activation(out=gshA, in_=gshA, func=AF.Exp), reads=["gsh"], writes=["gsh"])
    S.add("dve", lambda e: e.tensor_reduce(out=gsumA, in_=gshA, axis=AX.X, op=ALU.add), reads=["gsh"], writes=["gsum"])
    S.add("dve", lambda e: e.reciprocal(out=ptopA, in_=gsumA), reads=["gsum"], writes=["ptop"])
    S.add("dve", lambda e: e.tensor_scalar(out=gmaskA, in0=gmaskA, scalar1=-1.0, scalar2=1e30, op0=ALU.add, op1=ALU.mult),
          reads=["gmask"], writes=["gmask"])
    for g in range(4):
        S.add("dve", lambda e, g=g: e.tensor_tensor(out=elmA[:, :, 4 * g:4 * g + 4], in0=EL[:, :, 4 * g:4 * g + 4],
                                                    in1=gmaskA[:, :, g:g + 1].to_broadcast([128, NST, 4]), op=ALU.add),
              reads=["LG", "gmask"], writes=[f"elm{g}"])
    for st in range(NST):
        S.add("dve", lambda e, st=st: e.max(out=mx8A[:, st, :], in_=elmA[:, st, :]), reads=[f"elm{g}" for g in range(4)], writes=[f"mx8_{st % 4}"])
    mxr = [f"mx8_{k}" for k in range(4)]
    elr = [f"elm{g}" for g in range(4)]
    S.add("dve", lambda e: e.tensor_tensor(out=ddA, in0=mx8A[:, :, 1], in1=mx8A[:, :, 0], op=ALU.subtract), reads=mxr, writes=["dd"])
    S.add("act", lambda e: e.activation(out=ddA, in_=ddA, func=AF.Exp), reads=["dd"], writes=["dd"])
    S.add("dve", lambda e: e.tensor_scalar(out=ddA, in0=ddA, scalar1=1.0, scalar2=None, op0=ALU.add), reads=["dd"], writes=["dd"])
    S.add("dve", lambda e: e.reciprocal(out=ddA, in_=ddA), reads=["dd"], writes=["dd"])
    S.add("dve", lambda e: e.tensor_tensor(out=W1, in0=ptopA, in1=ddA, op=ALU.mult), reads=["ptop", "dd"], writes=["W1"])
    S.add("dve", lambda e: e.tensor_tensor(out=W2, in0=ptopA, in1=W1, op=ALU.subtract), reads=["ptop", "W1"], writes=["W2"])
    S.add("dve", lambda e: e.tensor_tensor(out=EQ1, in0=elmA, in1=mx8A[:, :, 0:1].to_broadcast([128, NST, 16]), op=ALU.is_equal),
          reads=elr + mxr, writes=["EQ1"])
    S.add("dve", lambda e: e.tensor_tensor(out=EQ2, in0=elmA, in1=mx8A[:, :, 1:2].to_broadcast([128, NST, 16]), op=ALU.is_equal),
          reads=elr + mxr, writes=["EQ2"])

    NF = NST * 16
    Mall = A.alloc([NF], BF16)
    Lmat = A.alloc([128], BF16)
    Lf = A.alloc([128], F32)
    onesm = A.alloc([128], BF16)
    within = A.alloc([NST, 16], F32)
    tot = A.alloc([NST, 16], F32)
    incl = A.alloc([NST, 16], F32)
    ones64 = A.alloc([max(NST, 16)], F32)
    q = A.alloc([16], F32)
    qi = A.alloc([16], I32)
    qf = A.alloc([16], F32)
    qg = A.alloc([16], F32)
    endc = A.alloc([16], F32)
    base = A.alloc([16], F32)
    tmp3 = A.alloc([NST, 16], F32)
    posf = A.alloc([NST], F32)
    jlim = A.alloc([NJ], F32)
    cmpj = A.alloc([NJ, 16], F32)
    ejf = A.alloc([NJ], F32)
    iop = A.alloc([1], F32)
    NZ = NSLOT // 128
    zt = A.alloc([NZ, 8], I32)
    tokid = A.alloc([NST, 8], I32)

    S.add("dve", lambda e: e.tensor_tensor(out=Mall, in0=EQ1.rearrange("p a b -> p (a b)"), in1=EQ2.rearrange("p a b -> p (a b)"), op=ALU.add),
          reads=["EQ1", "EQ2"], writes=["Mall"])
    S.add("pool", lambda e: e.affine_select(out=Lf, in_=onesf, pattern=[[1, 128]], compare_op=ALU.is_gt, fill=0.0, base=0, channel_multiplier=-1),
          reads=[], writes=["Lf"])
    S.add("dve", lambda e: e.tensor_copy(out=Lmat, in_=Lf), reads=["Lf"], writes=["Lmat"])
    S.add("dve", lambda e: e.tensor_copy(out=onesm, in_=onesf), reads=[], writes=["onesm"])
    S.add("dve", lambda e: e.memset(ones64, 1.0), writes=["ones64"])
    S.add("dve", lambda e: e.memset(zt, 0), writes=["zt"])
    S.add("pool", lambda e: e.iota(tokid, pattern=[[128, NST], [0, 8]], base=0, channel_multiplier=1), writes=["tokid"])
    S.add("pool", lambda e: e.iota(jlim, pattern=[[512, NJ]], base=0, channel_multiplier=0, allow_small_or_imprecise_dtypes=True), writes=["jlim"])
    S.add("pool", lambda e: e.iota(iop, pattern=[[0, 1]], base=0, channel_multiplier=1, allow_small_or_imprecise_dtypes=True), writes=["iop"])
    S.add("sp", lambda e: e.dma_start(out=tokof.rearrange("(a p) c -> p a c", p=128), in_=zt), reads=["zt"], writes=["tokof"], tag="tokz")
    wf = within.rearrange("p a b -> p (a b)")
    tf = tot.rearrange("p a b -> p (a b)")
    for (lhs, lhsn, dst, dstn) in ((Lmat, "Lmat", wf, "within"), (onesm, "onesm", tf, "tot")):
        for h in range((NF + 511) // 512):
            bk = 3 + h % 2
            lo = h * 512
            hi = min(NF, lo + 512)
            S.add("pe", lambda e, lhs=lhs, lo=lo, hi=hi, bk=bk: e.matmul(banks[bk][:, 0:hi - lo], lhsT=lhs, rhs=Mall[:, lo:hi], start=True, stop=True),
                  reads=[lhsn, "Mall"], writes=[f"bank{bk}"])
            S.add("dve", lambda e, dst=dst, lo=lo, hi=hi, bk=bk: e.tensor_copy(out=dst[:, lo:hi], in_=banks[bk][:, 0:hi - lo]),
                  reads=[f"bank{bk}"], writes=[dstn])
    for ex in range(16):
        S.add("dve", lambda e, ex=ex: e.tensor_tensor_scan(out=incl[:, :, ex], data0=ones64[:, 0:NST], data1=tot[:, :, ex], initial=0.0,
                                                           op0=ALU.mult, op1=ALU.add),
              reads=["tot", "ones64"], writes=["incl"])
    cnt = incl[:, NST - 1, :]
    S.add("dve", lambda e: e.tensor_scalar(out=q, in0=cnt, scalar1=511.0, scalar2=1.0 / 512, op0=ALU.add, op1=ALU.mult), reads=["incl"], writes=["q"])
    S.add("dve", lambda e: e.tensor_copy(out=qi, in_=q), reads=["q"], writes=["qi"])
    S.add("dve", lambda e: e.tensor_copy(out=qf, in_=qi), reads=["qi"], writes=["qf"])
    S.add("dve", lambda e: e.tensor_tensor(out=qg, in0=qf, in1=q, op=ALU.is_gt), reads=["qf", "q"], writes=["qg"])
    S.add("dve", lambda e: e.tensor_tensor(out=qf, in0=qf, in1=qg, op=ALU.subtract), reads=["qf", "qg"], writes=["qf"])
    S.add("dve", lambda e: e.tensor_scalar(out=qf, in0=qf, scalar1=512.0, scalar2=None, op0=ALU.mult), reads=["qf"], writes=["qf"])
    S.add("dve", lambda e: e.tensor_tensor_scan(out=endc, data0=ones64[:, 0:16], data1=qf, initial=0.0, op0=ALU.mult, op1=ALU.add),
          reads=["qf", "ones64"], writes=["endc"])
    S.add("dve", lambda e: e.tensor_tensor(out=base, in0=endc, in1=qf, op=ALU.subtract), reads=["endc", "qf"], writes=["base"])
    S.add("dve", lambda e: e.tensor_tensor(out=incl, in0=incl, in1=tot, op=ALU.subtract), reads=["incl", "tot", "q"], writes=["incl"])
    S.add("dve", lambda e: e.tensor_tensor(out=within, in0=within, in1=incl, op=ALU.add), reads=["within", "incl"], writes=["within"])
    S.add("dve", lambda e: e.tensor_tensor(out=within, in0=within, in1=base.unsqueeze(1).to_broadcast([128, NST, 16]), op=ALU.add),
          reads=["within", "base"], writes=["within"])
    for (EQ, eqn, posi, posn) in ((EQ1, "EQ1", pos1i, "pos1i"), (EQ2, "EQ2", pos2i, "pos2i")):
        S.add("dve", lambda e, EQ=EQ: e.tensor_tensor(out=tmp3, in0=EQ, in1=within, op=ALU.mult), reads=[eqn, "within"], writes=["tmp3"])
        S.add("dve", lambda e: e.tensor_reduce(out=posf, in_=tmp3, axis=AX.X, op=ALU.add), reads=["tmp3"], writes=["posf"])
        S.add("dve", lambda e, posi=posi: e.tensor_copy(out=posi, in_=posf), reads=["posf"], writes=[posn])
    S.add("dve", lambda e: e.tensor_tensor(out=cmpj, in0=endc.unsqueeze(1).to_broadcast([128, NJ, 16]),
                                           in1=jlim.unsqueeze(2).to_broadcast([128, NJ, 16]), op=ALU.is_le),
          reads=["endc", "jlim"], writes=["cmpj"])
    S.add("dve", lambda e: e.tensor_reduce(out=ejf, in_=cmpj, axis=AX.X, op=ALU.add), reads=["cmpj"], writes=["ejf"])
    S.add("dve", lambda e: e.tensor_scalar(out=ejf, in0=ejf, scalar1=15.0, scalar2=128.0, op0=ALU.min, op1=ALU.mult), reads=["ejf"], writes=["ejf"])
    S.add("dve", lambda e: e.tensor_scalar(out=ejf, in0=ejf, scalar1=iop[:, 0:1], scalar2=None, op0=ALU.add), reads=["ejf", "iop"], writes=["ejf"])
    S.add("dve", lambda e: e.tensor_copy(out=widx, in_=ejf), reads=["ejf"], writes=["widx"])
    sc_regions = []
    for st in range(NST):
        for (posi, posn) in ((pos1i, "pos1i"), (pos2i, "pos2i")):
            rn = f"tsc{st}{posn}"
            sc_regions.append(rn)
            S.add("pool", lambda e, st=st, posi=posi: e.indirect_dma_start(
                out=tokof, out_offset=bass.IndirectOffsetOnAxis(ap=posi[:, st:st + 1], axis=0), in_=tokid[:, st, :], in_offset=None),
                reads=[posn, "tokid", "tokof"], writes=[rn], tag="tsc")

    A.off = off_2a_tmp
    off_2d = A.off
    wbuf = [A.alloc([WROW], BF16) for _ in range(2)]
    Yt = [A.alloc([4, D], F32) for _ in range(2)]
    sg = [A.alloc([T2], BF16) for _ in range(2)]
    he = [A.alloc([4, T2], BF16) for _ in range(2)]
    tk = [A.alloc([4, 8], I32) for _ in range(3)]
    gu_ctr = [0]

    def next_gu():
        b = (2, 3, 4, 5)[gu_ctr[0] % 4]
        gu_ctr[0] += 1
        return banks[b][:, :], f"bank{b}"

    dn_ctr = [0]

    def next_dn():
        b = (6, 7, 4, 5)[dn_ctr[0] % 4]
        dn_ctr[0] += 1
        return banks[b][:, :], f"bank{b}"

    ev_ctr = [0]
    S.barrier()

    def loads_x(j):
        b = j % 3
        X = x1t[b]
        S.add("sp", lambda e, b=b, j=j: e.dma_start(out=tk[b], in_=tokof[j * 512:(j + 1) * 512, :].rearrange("(s p) c -> p s c", p=128)),
              writes=[f"tk{b}"], tag=f"tk{b}")
        for s in range(4):
            rn = f"xg{b}_{s}"
            S.add("pool", lambda e, b=b, s=s, X=X: e.indirect_dma_start(
                out=X[:, s, :], out_offset=None, in_=x1s, in_offset=bass.IndirectOffsetOnAxis(ap=tk[b][:, s, 0:1], axis=0)),
                reads=[f"tk{b}"], writes=[rn], tag=rn)

    def loads_w(j):
        b = j % 2
        S.add("pool", lambda e, b=b, j=j: e.indirect_dma_start(
            out=wbuf[b], out_offset=None, in_=wall, in_offset=bass.IndirectOffsetOnAxis(ap=widx[:, j:j + 1], axis=0)),
            reads=["widx"], writes=[f"wbuf{b}"], tag=f"wbuf{b}")

    def normA(j):
        b = j % 3
        X = x1t[b]
        xn = [f"xg{b}_{s}" for s in range(4)]
        for s in range(4):
            S.add("act", lambda e, s=s, X=X: e.activation(out=junk2, in_=X[:, s, :], func=AF.Square, accum_out=ssq2[:, s:s + 1]),
                  reads=[xn[s]], writes=["ssq2"])
        S.add("act", lambda e: e.activation(out=std2, in_=ssq2, func=AF.Sqrt, scale=1.0 / D, bias=EPS),
              reads=["ssq2"], writes=["std2"])
        S.add("dve", lambda e: e.reciprocal(out=rstd2, in_=std2), reads=["std2"], writes=["rstd2"])
        for s in range(4):
            S.add("dve", lambda e, s=s, X=X: e.tensor_scalar(out=hn4[s], in0=X[:, s, :], scalar1=rstd2[:, s:s + 1],
                                                            scalar2=None, op0=ALU.mult),
                  reads=[xn[s], "rstd2"], writes=[f"hn4_{s}"])

    def transT(j):
        hb = j % 2
        for s in range(4):
            tb = s % 2
            tp = banks[tb][:].bitcast(BF16).rearrange("p (a b) -> p a b", a=8)
            for kc in range(8):
                S.add("pe", lambda e, kc=kc, s=s, tp=tp: e.transpose(out=tp[:, kc, :], in_=hn4[s][:, kc * 128:(kc + 1) * 128], identity=ident),
                      reads=[f"hn4_{s}"], writes=[f"bank{tb}"])
            S.add("dve", lambda e, tp=tp, s=s, hb=hb: e.tensor_tensor(out=h2Ts[hb][:, :, s * 128:(s + 1) * 128], in0=tp, in1=g2bc, op=ALU.mult),
                  reads=[f"bank{tb}"], writes=[f"h2T{hb}"])

    def gu(j):
        b = j % 2
        h2T = h2Ts[j % 2]
        h2n = f"h2T{j % 2}"
        wgv = wbuf[b][:, 0:4096].rearrange("p (k n) -> p k n", k=8)
        wuv = wbuf[b][:, 4096:8192].rearrange("p (k n) -> p k n", k=8)
        for hc in range(4):
            pg, pgn = next_gu()
            pu, pun = next_gu()
            for kc in range(8):
                S.add("pe", lambda e, pg=pg, kc=kc, hc=hc, wgv=wgv, h2T=h2T: e.matmul(pg, lhsT=wgv[:, kc, hc * 128:(hc + 1) * 128], rhs=h2T[:, kc, :],
                                                                            start=(kc == 0), stop=(kc == 7)),
                      reads=[f"wbuf{b}", h2n], writes=[pgn])
            for kc in range(8):
                S.add("pe", lambda e, pu=pu, kc=kc, hc=hc, wuv=wuv, h2T=h2T: e.matmul(pu, lhsT=wuv[:, kc, hc * 128:(hc + 1) * 128], rhs=h2T[:, kc, :],
                                                                            start=(kc == 0), stop=(kc == 7)),
                      reads=[f"wbuf{b}", h2n], writes=[pun])
            sb = hc % 2
            S.add("act", lambda e, pg=pg, sb=sb: e.activation(out=sg[sb], in_=pg, func=AF.Silu), reads=[pgn], writes=[f"sg{sb}"])
            S.add("dve", lambda e, pu=pu, sb=sb, b=b, hc=hc: e.tensor_tensor(out=he[b][:, hc, :], in0=sg[sb], in1=pu, op=ALU.mult),
                  reads=[pun, f"sg{sb}"], writes=[f"he{b}_{hc}"])

    def down(j):
        b = j % 2
        wdv = wbuf[b][:, 8192:12288].rearrange("p (k n) -> p k n", k=4)
        for s in range(4):
            for half in range(2):
                pd, pdn = next_dn()
                for hc in range(4):
                    S.add("pe", lambda e, pd=pd, hc=hc, s=s, b=b, half=half, wdv=wdv: e.matmul(
                        pd, lhsT=he[b][:, hc, s * 128:(s + 1) * 128], rhs=wdv[:, hc, half * 512:(half + 1) * 512],
                        start=(hc == 0), stop=(hc == 3)),
                        reads=[f"he{b}_{hc}", f"wbuf{b}"], writes=[pdn])
                dst = Yt[b][:, s, half * 512:(half + 1) * 512]
                if ev_ctr[0] % 2 == 0:
                    S.add("act", lambda e, pd=pd, dst=dst: e.activation(out=dst, in_=pd, func=AF.Copy), reads=[pdn], writes=[f"Yt{b}_{s}{half}"])
                else:
                    S.add("dve", lambda e, pd=pd, dst=dst: e.tensor_copy(out=dst, in_=pd), reads=[pdn], writes=[f"Yt{b}_{s}{half}"])
                ev_ctr[0] += 1
        S.add("sp", lambda e, b=b, j=j: e.dma_start(out=Ys[j * 512:(j + 1) * 512, :].rearrange("(s p) d -> p s d", p=128), in_=Yt[b]),
              reads=[f"Yt{b}_{s}{h}" for s in range(4) for h in range(2)], writes=[f"Ys{j}"], tag=f"yo{b}")

    loads_x(0)
    loads_w(0)
    if NJ > 1:
        loads_x(1)
        loads_w(1)
    normA(0)
    transT(0)
    for j in range(NJ):
        if j + 2 < NJ:
            loads_x(j + 2)
        if j + 1 < NJ:
            normA(j + 1)
        gu(j)
        if j + 1 < NJ:
            transT(j + 1)
        down(j)
        if j + 2 < NJ:
            loads_w(j + 2)

    S.barrier()
    A.off = off_2d
    NB3 = 6
    C0 = [A.alloc([D], F32) for _ in range(NB3)]
    C1 = [A.alloc([D], F32) for _ in range(NB3)]
    C2 = [A.alloc([D], F32) for _ in range(NB3)]
    ssq3 = [A.alloc([1], F32) for _ in range(NB3)]
    std3 = [A.alloc([1], F32) for _ in range(NB3)]
    rstd3 = [A.alloc([1], F32) for _ in range(NB3)]

    def loads_d(st):
        b = st % NB3
        S.add("sp", lambda e, b=b, st=st: e.dma_start(out=C0[b], in_=x1s[st * 128:(st + 1) * 128, :]), writes=[f"C0{b}"], tag=f"c0{b}")
        S.add("pool", lambda e, b=b, st=st: e.indirect_dma_start(
            out=C1[b], out_offset=None, in_=Ys, in_offset=bass.IndirectOffsetOnAxis(ap=pos1i[:, st:st + 1], axis=0)),
            writes=[f"C1{b}"], tag=f"c1{b}")
        S.add("pool", lambda e, b=b, st=st: e.indirect_dma_start(
            out=C2[b], out_offset=None, in_=Ys, in_offset=bass.IndirectOffsetOnAxis(ap=pos2i[:, st:st + 1], axis=0)),
            writes=[f"C2{b}"], tag=f"c2{b}")

    def d_ab(st):
        b = st % NB3
        S.add("dve", lambda e, b=b, st=st: e.scalar_tensor_tensor(out=C0[b], in0=C1[b], scalar=W1[:, st:st + 1], in1=C0[b], op0=ALU.mult, op1=ALU.add),
              reads=[f"C1{b}", f"C0{b}"], writes=[f"C0{b}"])
        S.add("dve", lambda e, b=b, st=st: e.scalar_tensor_tensor(out=C0[b], in0=C2[b], scalar=W2[:, st:st + 1], in1=C0[b], op0=ALU.mult, op1=ALU.add),
              reads=[f"C2{b}", f"C0{b}"], writes=[f"C0{b}"])
        S.add("act", lambda e, b=b: e.activation(out=junk2, in_=C0[b], func=AF.Square, accum_out=ssq3[b]), reads=[f"C0{b}"], writes=[f"ssq3{b}"])
        S.add("act", lambda e, b=b: e.activation(out=std3[b], in_=ssq3[b], func=AF.Sqrt, scale=1.0 / D, bias=EPS), reads=[f"ssq3{b}"], writes=[f"std3{b}"])

    def d_fin(st):
        b = st % NB3
        S.add("dve", lambda e, b=b: e.reciprocal(out=rstd3[b], in_=std3[b]), reads=[f"std3{b}"], writes=[f"rstd3{b}"])
        S.add("dve", lambda e, b=b: e.scalar_tensor_tensor(out=C0[b], in0=C0[b], scalar=rstd3[b][:, 0:1], in1=gfbc, op0=ALU.mult, op1=ALU.mult),
              reads=[f"C0{b}", f"rstd3{b}"], writes=[f"C0{b}"])
        S.add("sp", lambda e, b=b, st=st: e.dma_start(out=out[st * 128:(st + 1) * 128, :], in_=C0[b]), reads=[f"C0{b}"], writes=[f"out{b}"], tag=f"co{b}")

    for st in range(min(NB3, NST)):
        loads_d(st)
    d_ab(0)
    for st in range(NST):
        if st + 1 < NST:
            d_ab(st + 1)
        d_fin(st)
        if st + NB3 < NST:
            loads_d(st + NB3)
    S.add("sp", None, reads=[f"out{b}" for b in range(NB3)])


def build_nc(n_seq=2, seq_len=4096, stop_after_phase1=False, sparse=True):
    T = n_seq * seq_len
    T1 = 256
    NT1 = T // T1
    TPS1 = seq_len // T1
    T2 = 512
    NT2 = T // T2
    NE = 16

    nc = bass.Bass("TRN2", target_bir_lowering=False)

    def din(name, shape):
        return nc.dram_tensor(name, shape, F32, kind="ExternalInput").ap()

    x = din("x", [T, D])
    norm1_g = din("norm1_g", [D])
    w_in = din("w_in", [D, 4608])
    pool_w = din("pool_w", [4, 128, 128])
    pool_scale = din("pool_scale", [512])
    conv_w = din("conv_w", [4, D])
    conv_b = din("conv_b", [D])
    rg_w_r = din("rg_w_r", [16, 64, 64])
    rg_b_r = din("rg_b_r", [D])
    rg_w_i = din("rg_w_i", [16, 64, 64])
    rg_b_i = din("rg_b_i", [D])
    rg_lambda = din("rg_lambda", [D])
    proj_a = din("proj_a", [512, D])
    proj_b = din("proj_b", [D, D])
    w_out = din("w_out", [D, D])
    norm2_g = din("norm2_g", [D])
    router_group_w = din("router_group_w", [D, 4])
    router_group_b = din("router_group_b", [4])
    router_expert_w = din("router_expert_w", [D, 16])
    router_expert_b = din("router_expert_b", [16])
    exp_w_gate = din("exp_w_gate", [NE, D, 512])
    exp_w_up = din("exp_w_up", [NE, D, 512])
    exp_w_down = din("exp_w_down", [NE, 512, D])
    norm_f_g = din("norm_f_g", [D])
    out = nc.dram_tensor("out", [T, D], F32, kind="ExternalOutput").ap()

    w_in_bf = nc.dram_tensor("w_in_bf", [9, 128, 8, 512], BF16, kind="Internal").ap()
    WROW = 12288
    wall = nc.dram_tensor("wall", [NE * 128, WROW], BF16, kind="Internal").ap()
    wall_v = wall.rearrange("(e p) n -> e p n", p=128)

    class _WV:
        def __init__(self, lo, k):
            self.lo, self.k = lo, k

        def __getitem__(self, e):
            return wall_v[e][:, self.lo:self.lo + 4096].rearrange("p (k n) -> p k n", k=self.k)

    wg_bf = _WV(0, 8)
    wu_bf = _WV(4096, 8)
    wd_bf = _WV(8192, 4)
    NJ = (2 * T) // 512 + NE
    NSLOT = NJ * 512
    I32 = mybir.dt.int32
    tokof = nc.dram_tensor("tokof", [NSLOT, 8], I32, kind="Internal").ap()
    Ys = nc.dram_tensor("Ys", [NSLOT, D], F32, kind="Internal").ap()
    x1s = nc.dram_tensor("x1s", [T, D], F32, kind="Internal").ap()

    es = ExitStack()
    NA = 105000
    arena_t = es.enter_context(nc.sbuf_tensor("arena", [128, NA], BF16))
    banks = [es.enter_context(nc.psum_tensor(f"bank{i}", [128, 512], F32)) for i in range(8)]
    A = Arena(arena_t, NA)
    S = Sched(nc)

    def vcol(v):
        return v.rearrange("(k p) -> p k", p=128)

    ident = A.alloc([128], BF16)
    identf = A.alloc([128], F32)
    onesf = A.alloc([128], F32)
    ones_bf = A.alloc([128], BF16)
    g1T = A.alloc([8], F32)
    g2T = A.alloc([8], F32)
    g1bc = A.alloc([8, 128], BF16)
    g2bc = A.alloc([8, 128], BF16)
    psT = A.alloc([4], F32)
    cwT = A.alloc([4, 8], F32)
    cbT = A.alloc([8], F32)
    hbr = A.alloc([8], F32)
    hbi = A.alloc([8], F32)
    lamT = A.alloc([8], F32)
    chalf = A.alloc([8], F32)
    invc = A.alloc([16], F32)
    gfrow = A.alloc([D], F32)
    gfbc = A.alloc([D], F32)
    rbrow = A.alloc([32], F32)
    rb_bf = A.alloc([32], BF16)
    rw = A.alloc([8, 20], BF16)
    const_end = A.off

    def small_load(dst, src, name):
        S.add("sp", lambda e: e.dma_start(out=dst, in_=src, allow_slow_non_contiguous=True),
              writes=[name], tag=name)

    small_load(g1T, vcol(norm1_g), "g1T")
    small_load(g2T, vcol(norm2_g), "g2T")
    small_load(psT, pool_scale.rearrange("(k p) -> p k", p=128), "psT")
    small_load(cwT, conv_w.rearrange("k (c p) -> p k c", p=128), "cwT")
    small_load(cbT, vcol(conv_b), "cbT")
    small_load(hbr, vcol(rg_b_r), "hbr")
    small_load(hbi, vcol(rg_b_i), "hbi")
    small_load(lamT, vcol(rg_lambda), "lamT")
    S.add("sp", lambda e: e.dma_start(out=gfrow[0:1, :], in_=norm_f_g.rearrange("(o n) -> o n", o=1)),
          writes=["gfrow"], tag="gfrow")
    S.add("sp", lambda e: e.dma_start(out=rbrow[0:1, 0:4], in_=router_group_b.rearrange("(o n) -> o n", o=1)),
          writes=["rbrow_a"], tag="rbrow_a")
    S.add("sp", lambda e: e.dma_start(out=rbrow[0:1, 4:20], in_=router_expert_b.rearrange("(o n) -> o n", o=1)),
          writes=["rbrow_b"], tag="rbrow_b")
    S.add("pool", lambda e: e.dma_start(out=rw[:, :, 0:4], in_=router_group_w.rearrange("(k p) n -> p k n", p=128)),
          writes=["rw_a"], tag="rw_a")
    S.add("pool", lambda e: e.dma_start(out=rw[:, :, 4:20], in_=router_expert_w.rearrange("(k p) n -> p k n", p=128)),
          writes=["rw_b"], tag="rw_b")

    S.add("pool", lambda e: e.memset(onesf, 1.0), writes=["onesf"])
    S.add("pool", lambda e: e.affine_select(out=identf, in_=onesf, pattern=[[-1, 128]], compare_op=ALU.is_equal,
                                            fill=0.0, base=0, channel_multiplier=1),
          reads=["onesf"], writes=["identf"])
    S.add("dve", lambda e: e.tensor_copy(out=ident, in_=identf), reads=["identf"], writes=["ident"])
    S.add("dve", lambda e: e.tensor_copy(out=ones_bf, in_=onesf), reads=["onesf"], writes=["ones_bf"])
    S.add("dve", lambda e: e.tensor_copy(out=g1bc, in_=g1T.unsqueeze(2).to_broadcast([128, 8, 128])),
          reads=["g1T"], writes=["g1bc"])
    S.add("dve", lambda e: e.tensor_copy(out=g2bc, in_=g2T.unsqueeze(2).to_broadcast([128, 8, 128])),
          reads=["g2T"], writes=["g2bc"])
    S.add("dve", lambda e: e.tensor_copy(out=rb_bf[0:1, 0:20], in_=rbrow[0:1, 0:20]),
          reads=["rbrow_a", "rbrow_b"], writes=["rb_bf"])
    S.add("dve", lambda e: e.tensor_scalar(out=hbr, in0=hbr, scalar1=0.5, scalar2=None, op0=ALU.mult),
          reads=["hbr"], writes=["hbr"])
    S.add("dve", lambda e: e.tensor_scalar(out=hbi, in0=hbi, scalar1=0.5, scalar2=None, op0=ALU.mult),
          reads=["hbi"], writes=["hbi"])
    sp_a = A.alloc([8], F32)
    sp_b = A.alloc([8], F32)
    S.add("dve", lambda e: e.tensor_scalar(out=sp_b, in0=lamT, scalar1=-1.0, scalar2=None, op0=ALU.mult),
          reads=["lamT"], writes=["sp_b"])
    S.add("dve", lambda e: e.tensor_tensor(out=sp_a, in0=lamT, in1=sp_b, op=ALU.max),
          reads=["lamT", "sp_b"], writes=["sp_a"])
    S.add("act", lambda e: e.activation(out=sp_a, in_=sp_a, func=AF.Exp, scale=-1.0), reads=["sp_a"], writes=["sp_a"])
    S.add("act", lambda e: e.activation(out=sp_a, in_=sp_a, func=AF.Ln, bias=1.0), reads=["sp_a"], writes=["sp_a"])
    S.add("dve", lambda e: e.tensor_scalar(out=sp_b, in0=lamT, scalar1=-1.0, scalar2=0.0, op0=ALU.mult, op1=ALU.max),
          reads=["lamT"], writes=["sp_b"])
    S.add("dve", lambda e: e.tensor_tensor(out=sp_a, in0=sp_a, in1=sp_b, op=ALU.add), reads=["sp_a", "sp_b"], writes=["sp_a"])
    S.add("dve", lambda e: e.tensor_scalar(out=chalf, in0=sp_a, scalar1=-4.0, scalar2=None, op0=ALU.mult),
          reads=["sp_a"], writes=["chalf"])
    for t in range(16):
        S.add("pool", lambda e, t=t: e.memset(invc[:, t:t + 1], 1.0 / (t + 1)), writes=["invc"])
    for h in range(2):
        S.add("pe", lambda e, h=h: e.matmul(banks[h][:, :], lhsT=onesf[0:1, :], rhs=gfrow[0:1, h * 512:(h + 1) * 512],
                                            start=True, stop=True),
              reads=["onesf", "gfrow"], writes=[f"bank{h}"])
        S.add("dve", lambda e, h=h: e.tensor_copy(out=gfbc[:, h * 512:(h + 1) * 512], in_=banks[h][:, :]),
              reads=[f"bank{h}"], writes=["gfbc"])

    for g in range(9):
        S.add("pool", lambda e, g=g: e.dma_start(out=w_in_bf[g], in_=w_in[:, g * 512:(g + 1) * 512].rearrange("(k p) n -> p k n", p=128)),
              writes=[f"w_in_bf{g}"], tag=f"w_in_bf{g}")

    A1_start = A.off
    pa_w = A.alloc([4, D], BF16)
    pb_w = A.alloc([8, D], BF16)
    wo_w = A.alloc([8, D], BF16)
    pl_w = A.alloc([4, 128], BF16)
    wr_bd = A.alloc([8, 128], BF16)
    wi_bd = A.alloc([8, 128], BF16)
    dg = A.alloc([4, 8, 128], BF16)
    S.add("pool", lambda e: e.dma_start(out=pl_w, in_=pool_w.rearrange("g c d -> c g d")), writes=["pl_w"], tag="pl_w")
    S.add("pool", lambda e: e.dma_start(out=pa_w, in_=proj_a.rearrange("(k p) n -> p k n", p=128)), writes=["pa_w"], tag="pa_w")
    S.add("dve", lambda e: e.memset(wr_bd, 0.0), writes=["wr_bd"])
    S.add("dve", lambda e: e.memset(wi_bd, 0.0), writes=["wi_bd"])
    for (wsrc, wdst, nm) in ((rg_w_r, wr_bd, "wr_bd"), (rg_w_i, wi_bd, "wi_bd")):
        for hh in range(2):
            src = wsrc.rearrange("(c two) a b -> two a c b", two=2)[hh]
            S.add("pool", lambda e, src=src, wdst=wdst, hh=hh: e.dma_start(
                out=wdst[hh * 64:(hh + 1) * 64, :, hh * 64:(hh + 1) * 64], in_=src),
                writes=[nm], tag=nm + str(hh))
    S.add("pool", lambda e: e.dma_start(out=pb_w, in_=proj_b.rearrange("(k p) n -> p k n", p=128)), writes=["pb_w"], tag="pb_w")
    S.add("pool", lambda e: e.dma_start(out=wo_w, in_=w_out.rearrange("(k p) n -> p k n", p=128)), writes=["wo_w"], tag="wo_w")
    for k in range(4):
        for c in range(8):
            S.add("dve", lambda e, k=k, c=c: e.tensor_scalar(out=dg[:, k, c, :], in0=identf, scalar1=cwT[:, k, c:c + 1],
                                                              scalar2=None, op0=ALU.mult),
                  reads=["identf", "cwT"], writes=["dg"])
    def cast_expert(e_, after):
        S.add("pool", lambda e, e_=e_: e.dma_start(out=wg_bf[e_], in_=exp_w_gate[e_].rearrange("(k p) n -> p k n", p=128)),
              reads=after, writes=[f"wg_bf{e_}"], tag="wg_bf")
        S.add("pool", lambda e, e_=e_: e.dma_start(out=wu_bf[e_], in_=exp_w_up[e_].rearrange("(k p) n -> p k n", p=128)),
              reads=after, writes=[f"wu_bf{e_}"], tag="wu_bf")
        S.add("pool", lambda e, e_=e_: e.dma_start(out=wd_bf[e_], in_=exp_w_down[e_].rearrange("(k p) n -> p k n", p=128)),
              reads=after, writes=[f"wd_bf{e_}"], tag="wd_bf")

    cast_state = [0]

    NXS = 6
    xs = [A.alloc([D], F32) for _ in range(NXS)]
    hn = [A.alloc([D], BF16) for _ in range(2)]
    ssq = [A.alloc([2], F32) for _ in range(2)]
    std = [A.alloc([2], F32) for _ in range(2)]
    rstd = [A.alloc([2], F32) for _ in range(2)]
    hT = [A.alloc([8, T1], BF16) for _ in range(2)]
    wb = [A.alloc([8, 512], BF16) for _ in range(3)]
    UPW = 16 + T1
    up = A.alloc([4, UPW], F32)
    s1 = A.alloc([UPW], F32)
    s2 = A.alloc([UPW], F32)
    ptmp = A.alloc([16], F32)
    z = A.alloc([4, T1], BF16)
    pa = A.alloc([4, T1], BF16)
    ULW = 3 + T1
    ul = A.alloc([8, ULW], BF16)
    gg2 = [A.alloc([8, T1], BF16) for _ in range(2)]
    tha2 = [A.alloc([8, T1], BF16) for _ in range(2)]
    thb2 = [A.alloc([8, T1], BF16) for _ in range(2)]
    xc = [A.alloc([T1], BF16) for _ in range(2)]
    thr = [A.alloc([T1], F32) for _ in range(2)]
    thi = [A.alloc([T1], F32) for _ in range(2)]
    a_t = A.alloc([8, T1], F32)
    t_t = A.alloc([8, T1], F32)
    mult = [A.alloc([T1], F32) for _ in range(2)]
    hs = [A.alloc([T1], F32) for _ in range(2)]
    hst = A.alloc([8], F32)
    hg = A.alloc([8, T1], BF16)
    m1 = A.alloc([8, T1], BF16)
    m2 = A.alloc([8, T1], BF16)
    A1_end = A.off

    slotA = [(b, 0) for b in (2, 3, 4, 5)]
    slot_ctr = [0]

    def next_slot():
        b, h = slotA[slot_ctr[0] % len(slotA)]
        slot_ctr[0] += 1
        return banks[b][:, h * 256:(h + 1) * 256], f"bank{b}"

    bigB = [6, 7]
    big_ctr = [0]

    def next_big():
        b = bigB[big_ctr[0] % 2]
        big_ctr[0] += 1
        return banks[b][:, :], f"bank{b}"

    x_t = x.rearrange("(n p) d -> n p d", p=128)
    x1_t = x1s.rearrange("(n p) d -> n p d", p=128)
    out_t = out.rearrange("(n p) d -> n p d", p=128)
    sub_ctr = [0]
    wb_ctr = [0]

    xbufs_of = {}

    def load_x(ti):
        bl = []
        for s in range(2):
            bi = sub_ctr[0] % NXS
            sub_ctr[0] += 1
            bl.append(bi)
            row = ti * 2 + s
            S.add("sp", lambda e, bi=bi, row=row: e.dma_start(out=xs[bi], in_=x_t[row]),
                  writes=[f"xs{bi}"], tag=f"xs{bi}")
        xbufs_of[ti] = bl

    GTOT = NT1 * 9

    def load_wb(G):
        wi_ = G % 3
        g = G % 9
        S.add("sp", lambda e, wi_=wi_, g=g: e.dma_start(out=wb[wi_], in_=w_in_bf[g]),
              reads=[f"w_in_bf{g}"], writes=[f"wb{wi_}"], tag=f"wb{wi_}")

    def stepA(ti, part="all"):
        par = ti % 2
        xbufs = xbufs_of[ti]
        for s in range(2 if part in ("all", "norm") else 0):
            bi = xbufs[s]
            S.add("act", lambda e, bi=bi, s=s, par=par: e.activation(out=hn[s], in_=xs[bi], func=AF.Square,
                                                                    accum_out=ssq[par][:, s:s + 1]),
                  reads=[f"xs{bi}"], writes=[f"ssq{par}", f"hn{s}"])
        if part in ("all", "norm"):
            S.add("act", lambda e, par=par: e.activation(out=std[par], in_=ssq[par], func=AF.Sqrt, scale=1.0 / D, bias=EPS),
                  reads=[f"ssq{par}"], writes=[f"std{par}"])
            S.add("dve", lambda e, par=par: e.reciprocal(out=rstd[par], in_=std[par]),
                  reads=[f"std{par}"], writes=[f"rstd{par}"])
            for s in range(2):
                bi = xbufs[s]
                S.add("dve", lambda e, bi=bi, s=s, par=par: e.tensor_scalar(
                    out=hn[s], in0=xs[bi], scalar1=rstd[par][:, s:s + 1], scalar2=None, op0=ALU.mult),
                    reads=[f"xs{bi}", f"rstd{par}"], writes=[f"hn{s}"])
        for s in range(2 if part in ("all", "T") else 0):
            hb = s
            tb = s
            tp = banks[tb][:].bitcast(BF16).rearrange("p (a b) -> p a b", a=8)
            for kc in range(8):
                S.add("pe", lambda e, kc=kc, hb=hb, tp=tp: e.transpose(out=tp[:, kc, :], in_=hn[hb][:, kc * 128:(kc + 1) * 128],
                                                                       identity=ident),
                      reads=[f"hn{hb}", "ident"], writes=[f"bank{tb}"])
            S.add("dve", lambda e, tp=tp, par=par, s=s: e.tensor_tensor(out=hT[par][:, :, s * 128:(s + 1) * 128], in0=tp, in1=g1bc,
                                                                        op=ALU.mult),
                  reads=[f"bank{tb}", "g1bc"], writes=[f"hT{par}"])

    def stepB(ti, groups, hook=None):
        par = ti % 2
        seq_start = (ti % TPS1 == 0)
        if seq_start and 0 in groups:
            S.add("dve", lambda e: e.memset(up[:, :, 0:16], 0.0), writes=[f"up{j}" for j in range(4)])
            S.add("dve", lambda e: e.memset(ul[:, :, 0:3], 0.0), writes=[f"ul{c}" for c in range(8)])
        for g in groups:
            G = ti * 9 + g
            wi_ = G % 3
            for j in range(4):
                oc = 4 * g + j
                ps, psn = next_slot()
                for kc in range(8):
                    S.add("pe", lambda e, ps=ps, wi_=wi_, kc=kc, j=j, par=par: e.matmul(
                        ps, lhsT=wb[wi_][:, kc, j * 128:(j + 1) * 128], rhs=hT[par][:, kc, :], start=(kc == 0), stop=(kc == 7)),
                        reads=[f"wb{wi_}", f"hT{par}"], writes=[psn])
                if oc < 4:
                    S.add("act", lambda e, ps=ps, oc=oc: e.activation(out=up[:, oc, 16:16 + T1], in_=ps, func=AF.Copy),
                          reads=[psn], writes=[f"up{oc}"])
                elif oc < 12:
                    c = oc - 4
                    S.add("act", lambda e, ps=ps, c=c: e.activation(out=ul[:, c, 3:3 + T1], in_=ps, func=AF.Copy),
                          reads=[psn], writes=[f"ul{c}"])
                elif oc < 20:
                    c = oc - 12
                    S.add("act", lambda e, ps=ps, c=c, par=par: e.activation(out=gg2[par][:, c, :], in_=ps, func=AF.Gelu_apprx_tanh),
                          reads=[psn], writes=[f"gg{par}_{c}"])
                elif oc < 28:
                    c = oc - 20
                    S.add("act", lambda e, ps=ps, c=c, par=par: e.activation(out=tha2[par][:, c, :], in_=ps, func=AF.Tanh, scale=0.5),
                          reads=[psn], writes=[f"tha{par}_{c}"])
                else:
                    c = oc - 28
                    S.add("act", lambda e, ps=ps, c=c, par=par: e.activation(out=thb2[par][:, c, :], in_=ps, func=AF.Tanh, scale=0.5),
                          reads=[psn], writes=[f"thb{par}_{c}"])
                if hook is not None:
                    hook(oc)
            if G + 3 < GTOT:
                load_wb(G + 3)

    def stepC(ti, part="all"):
        seq_start = (ti % TPS1 == 0)
        for j in range(4 if part in ("all", "dve") else 0):
            w = 2 << j
            U = up[:, j, :]
            cur = U
            curn = f"up{j}"
            d = 1
            tmps = [(s1, "s1"), (s2, "s2")]
            k = 0
            while d < w:
                dst, dstn = tmps[k % 2]
                lo = 2 * d - 1
                S.add("dve", lambda e, dst=dst, cur=cur, lo=lo, d=d: e.tensor_tensor(
                    out=dst[:, lo:UPW], in0=cur[:, lo:UPW], in1=cur[:, lo - d:UPW - d], op=ALU.add),
                    reads=[curn], writes=[dstn])
                cur, curn = dst, dstn
                d *= 2
                k += 1
            S.add("dve", lambda e, cur=cur, U=U, j=j, w=w: e.scalar_tensor_tensor(
                out=z[:, j, :], in0=cur[:, 16:UPW], scalar=1.0 / w, in1=U[:, 16:UPW], op0=ALU.mult, op1=ALU.subtract),
                reads=[curn, f"up{j}"], writes=[f"z{j}"])
            if seq_start:
                n = w - 1
                S.add("dve", lambda e, cur=cur, n=n: e.tensor_tensor(out=ptmp[:, 0:n], in0=cur[:, 16:16 + n], in1=invc[:, 0:n], op=ALU.mult),
                      reads=[curn, "invc"], writes=["ptmp"])
                S.add("dve", lambda e, U=U, n=n, j=j: e.tensor_tensor(out=z[:, j, 0:n], in0=ptmp[:, 0:n], in1=U[:, 16:16 + n], op=ALU.subtract),
                      reads=["ptmp", f"up{j}"], writes=[f"z{j}"])
            S.add("dve", lambda e, U=U: e.tensor_copy(out=U[:, 0:16], in_=U[:, T1:T1 + 16]),
                  reads=[f"up{j}"], writes=[f"up{j}"])
        for j in range(4 if part in ("all", "pe") else 0):
            ps, psn = next_slot()
            S.add("pe", lambda e, ps=ps, j=j: e.matmul(ps, lhsT=pl_w[:, j, :], rhs=z[:, j, :], start=True, stop=True),
                  reads=["pl_w", f"z{j}"], writes=[psn])
            S.add("act", lambda e, ps=ps, j=j: e.activation(out=pa[:, j, :], in_=ps, func=AF.Copy, scale=psT[:, j:j + 1]),
                  reads=[psn, "psT"], writes=[f"pa{j}"])

    def stepD(ti, between=None):
        slots = {}

        def s1(c):
            cb = c % 2
            ps, psn = next_slot()
            for k in range(4):
                S.add("pe", lambda e, ps=ps, k=k, c=c: e.matmul(ps, lhsT=dg[:, k, c, :], rhs=ul[:, c, k:k + T1],
                                                                start=(k == 0), stop=(k == 3)),
                      reads=["dg", f"ul{c}"], writes=[psn])
            S.add("dve", lambda e, ps=ps, c=c, cb=cb: e.tensor_scalar(out=xc[cb], in0=ps, scalar1=cbT[:, c:c + 1], scalar2=None, op0=ALU.add),
                  reads=[psn, "cbT"], writes=[f"xc{cb}"])
            S.add("dve", lambda e, c=c: e.tensor_copy(out=ul[:, c, 0:3], in_=ul[:, c, T1:T1 + 3]),
                  reads=[f"ul{c}"], writes=[f"ul{c}"])

        def s2(c):
            cb = c % 2
            psr, psrn = next_slot()
            S.add("pe", lambda e, psr=psr, c=c, cb=cb: e.matmul(psr, lhsT=wr_bd[:, c, :], rhs=xc[cb], start=True, stop=True),
                  reads=["wr_bd", f"xc{cb}"], writes=[psrn])
            psi, psin = next_slot()
            S.add("pe", lambda e, psi=psi, c=c, cb=cb: e.matmul(psi, lhsT=wi_bd[:, c, :], rhs=xc[cb], start=True, stop=True),
                  reads=["wi_bd", f"xc{cb}"], writes=[psin])
            S.add("act", lambda e, psr=psr, c=c, cb=cb: e.activation(out=thr[cb], in_=psr, func=AF.Tanh, scale=0.5, bias=hbr[:, c:c + 1]),
                  reads=[psrn, "hbr"], writes=[f"thr{cb}"])
            S.add("act", lambda e, psi=psi, c=c, cb=cb: e.activation(out=thi[cb], in_=psi, func=AF.Tanh, scale=0.5, bias=hbi[:, c:c + 1]),
                  reads=[psin, "hbi"], writes=[f"thi{cb}"])
            S.add("act", lambda e, c=c, cb=cb: e.activation(out=a_t[:, c, :], in_=thr[cb], func=AF.Exp,
                                                            scale=chalf[:, c:c + 1], bias=chalf[:, c:c + 1]),
                  reads=[f"thr{cb}", "chalf"], writes=[f"a{c}"])
            S.add("dve", lambda e, c=c, cb=cb: e.scalar_tensor_tensor(out=t_t[:, c, :], in0=thi[cb], scalar=1.0, in1=xc[cb],
                                                                      op0=ALU.add, op1=ALU.mult),
                  reads=[f"thi{cb}", f"xc{cb}"], writes=[f"t{c}"])

        s1(0)
        for c in range(8):
            if c + 1 < 8:
                s1(c + 1)
            if between is not None:
                between(c)
            s2(c)

    def stepE(ti, chunks=range(8)):
        par = ti % 2
        if ti % TPS1 == 0 and 0 in chunks:
            S.add("dve", lambda e: e.memset(hst, 0.0), writes=["hst"])
        for c in chunks:
            cb = c % 2
            S.add("act", lambda e, c=c, cb=cb: e.activation(out=mult[cb], in_=a_t[:, c, :], func=AF.Square),
                  reads=[f"a{c}"], writes=[f"mult{cb}"])
            S.add("act", lambda e, cb=cb: e.activation(out=mult[cb], in_=mult[cb], func=AF.Sqrt, scale=-0.25, bias=0.25),
                  reads=[f"mult{cb}"], writes=[f"mult{cb}"])
            S.add("dve", lambda e, c=c, cb=cb: e.tensor_tensor(out=t_t[:, c, :], in0=t_t[:, c, :], in1=mult[cb], op=ALU.mult),
                  reads=[f"t{c}", f"mult{cb}"], writes=[f"t{c}"])
            S.add("dve", lambda e, c=c, cb=cb: e.tensor_tensor_scan(out=hs[cb], data0=a_t[:, c, :], data1=t_t[:, c, :],
                                                                    initial=hst[:, c:c + 1], op0=ALU.mult, op1=ALU.add),
                  reads=[f"a{c}", f"t{c}", "hst"], writes=[f"hs{cb}"])
            S.add("dve", lambda e, c=c, cb=cb: e.tensor_copy(out=hst[:, c:c + 1], in_=hs[cb][:, T1 - 1:T1]),
                  reads=[f"hs{cb}"], writes=["hst"])
            S.add("dve", lambda e, c=c, cb=cb, par=par: e.tensor_tensor(out=hg[:, c, :], in0=hs[cb], in1=gg2[par][:, c, :], op=ALU.mult),
                  reads=[f"hs{cb}", f"gg{par}_{c}"], writes=[f"hg{c}"])

    def stepF(ti):
        par = ti % 2
        for oc in range(8):
            ps, psn = next_slot()
            for kc in range(4):
                S.add("pe", lambda e, ps=ps, kc=kc, oc=oc: e.matmul(ps, lhsT=pa_w[:, kc, oc * 128:(oc + 1) * 128], rhs=pa[:, kc, :],
                                                                    start=(kc == 0), stop=(kc == 3)),
                      reads=["pa_w", f"pa{kc}"], writes=[psn])
            S.add("dve", lambda e, ps=ps, oc=oc, par=par: e.scalar_tensor_tensor(out=m1[:, oc, :], in0=tha2[par][:, oc, :], scalar=1.0, in1=ps,
                                                                        op0=ALU.add, op1=ALU.mult),
                  reads=[psn, f"tha{par}_{oc}"], writes=[f"m1_{oc}"])
        for oc in range(8):
            ps, psn = next_slot()
            for kc in range(8):
                S.add("pe", lambda e, ps=ps, kc=kc, oc=oc: e.matmul(ps, lhsT=pb_w[:, kc, oc * 128:(oc + 1) * 128], rhs=hg[:, kc, :],
                                                                    start=(kc == 0), stop=(kc == 7)),
                      reads=["pb_w", f"hg{kc}"], writes=[psn])
            S.add("dve", lambda e, ps=ps, oc=oc, par=par: e.scalar_tensor_tensor(out=m2[:, oc, :], in0=thb2[par][:, oc, :], scalar=1.0, in1=ps,
                                                                        op0=ALU.add, op1=ALU.mult),
                  reads=[psn, f"thb{par}_{oc}"], writes=[f"m2_{oc}"])
            S.add("dve", lambda e, oc=oc: e.tensor_tensor(out=m1[:, oc, :], in0=m1[:, oc, :], in1=m2[:, oc, :], op=ALU.add),
                  reads=[f"m1_{oc}", f"m2_{oc}"], writes=[f"m1_{oc}"])

    def G_pieces(ti):
        xbufs = xbufs_of[ti]
        state = {}
        pieces = []
        for s in range(2):
            for half in range(2):
                for part in range(2):
                    def piece(s=s, half=half, part=part):
                        bi = xbufs[s]
                        row = ti * 2 + s
                        if part == 0:
                            state[(s, half)] = next_big()
                        ps, psn = state[(s, half)]
                        for kc in range(part * 4, part * 4 + 4):
                            S.add("pe", lambda e, ps=ps, kc=kc: e.matmul(
                                ps, lhsT=m1[:, kc, s * 128:(s + 1) * 128], rhs=wo_w[:, kc, half * 512:(half + 1) * 512],
                                start=(kc == 0), stop=(kc == 7)),
                                reads=["wo_w", f"m1_{kc}"], writes=[psn])
                        if part == 1:
                            S.add("dve", lambda e, ps=ps, bi=bi: e.scalar_tensor_tensor(
                                out=xs[bi][:, half * 512:(half + 1) * 512], in0=ps, scalar=0.5, in1=xs[bi][:, half * 512:(half + 1) * 512],
                                op0=ALU.mult, op1=ALU.add),
                                reads=[psn, f"xs{bi}"], writes=[f"xs{bi}"])
                            if half == 1:
                                dst = out_t[row] if stop_after_phase1 else x1_t[row]
                                S.add("sp", lambda e, bi=bi, dst=dst: e.dma_start(out=dst, in_=xs[bi]),
                                      reads=[f"xs{bi}"], writes=[f"x1o{bi}"], tag=f"xo{bi}")
                    pieces.append(piece)
        return pieces

    load_x(0)
    if NT1 > 1:
        load_x(1)
    for G in range(min(3, GTOT)):
        load_wb(G)
    stepA(0)
    stepB(0, list(range(9)))
    stepC(0)
    stepD(0)
    if NT1 > 1:
        stepA(1, "norm")
    for ti in range(NT1):
        nxt = ti + 1 < NT1
        if nxt:
            stepA(ti + 1, "T")
        if ti >= 1 and cast_state[0] < NE:
            cast_expert(cast_state[0], [f"hT{(ti + 1) % 2}"])
            cast_state[0] += 1
        if ti + 2 < NT1:
            load_x(ti + 2)
        if nxt:
            def hook(oc, ti=ti):
                if oc % 2 == 1 and oc // 2 < 8:
                    stepE(ti, [oc // 2])
            stepE(ti, [])
            stepB(ti + 1, list(range(9)), hook=hook)
        else:
            stepE(ti)
        if ti >= 1:
            stepC(ti, "pe")
        stepF(ti)
        if ti + 2 < NT1:
            stepA(ti + 2, "norm")
        pcs = G_pieces(ti)
        if nxt:
            stepD(ti + 1, between=lambda c: pcs[c]())
            stepC(ti + 1, "dve")
        else:
            for p_ in pcs:
                p_()

    while cast_state[0] < NE:
        cast_expert(cast_state[0], [])
        cast_state[0] += 1
    if stop_after_phase1:
        S.add("sp", None, reads=[f"x1o{b}" for b in range(NXS)])
    elif sparse:
        build_sparse_phase2(locals())
    else:
        S.barrier()
        A.off = A1_start
        x1t = [A.alloc([4, D], F32) for _ in range(2)]
        junk2 = A.alloc([D], BF16)
        hn2 = [A.alloc([D], BF16) for _ in range(2)]
        ssq2 = A.alloc([4], F32)
        std2 = A.alloc([4], F32)
        rstd2 = A.alloc([4], F32)
        h2T = A.alloc([8, T2], BF16)
        lg = A.alloc([4, 20], F32)
        gmax = A.alloc([4], F32)
        gsh = A.alloc([4, 4], F32)
        gsum = A.alloc([4], F32)
        ptop = A.alloc([4], F32)
        gmask = A.alloc([4, 4], F32)
        elm = A.alloc([4, 16], F32)
        mx8 = A.alloc([4, 8], F32)
        dd = A.alloc([4], F32)
        w1 = A.alloc([4], F32)
        w2 = A.alloc([4], F32)
        eq1 = A.alloc([4, 16], F32)
        eq2 = A.alloc([4, 16], F32)
        comb = A.alloc([4, 16], F32)
        wgb = [A.alloc([8, 512], BF16) for _ in range(2)]
        wub = [A.alloc([8, 512], BF16) for _ in range(2)]
        wdb = [A.alloc([4, D], BF16) for _ in range(2)]
        sg = [A.alloc([T2], BF16) for _ in range(2)]
        he = [A.alloc([4, T2], BF16) for _ in range(2)]
        ssq3 = A.alloc([4], F32)
        std3 = A.alloc([4], F32)
        rstd3 = A.alloc([4], F32)

        gu_banks = [2, 3, 4, 5]
        gu_ctr = [0]

        def next_gu():
            b = gu_banks[gu_ctr[0] % 4]
            gu_ctr[0] += 1
            return banks[b][:, :], f"bank{b}"

        dn_ctr = [0]

        def next_dn():
            b = (6, 7)[dn_ctr[0] % 2]
            dn_ctr[0] += 1
            return banks[b][:, :], f"bank{b}"

        ectr = [0]
        for ti in range(NT2):
            xb = ti % 2
            X = x1t[xb]
            S.add("sp", lambda e, X=X, ti=ti: e.dma_start(out=X, in_=x1s[ti * T2:(ti + 1) * T2, :].rearrange("(s p) d -> p s d", p=128)),
                  reads=["x1s"], writes=[f"x1t{xb}"], tag=f"x1t{xb}")
            for s in range(4):
                S.add("act", lambda e, X=X, s=s: e.activation(out=junk2, in_=X[:, s, :], func=AF.Square, accum_out=ssq2[:, s:s + 1]),
                      reads=[f"x1t{xb}"], writes=["ssq2"])
            S.add("act", lambda e: e.activation(out=std2, in_=ssq2, func=AF.Sqrt, scale=1.0 / D, bias=EPS),
                  reads=["ssq2"], writes=["std2"])
            S.add("dve", lambda e: e.reciprocal(out=rstd2, in_=std2), reads=["std2"], writes=["rstd2"])
            for s in range(4):
                hb = s % 2
                S.add("dve", lambda e, X=X, s=s, hb=hb: e.tensor_scalar(out=hn2[hb], in0=X[:, s, :], scalar1=rstd2[:, s:s + 1],
                                                                        scalar2=None, op0=ALU.mult),
                      reads=[f"x1t{xb}", "rstd2"], writes=[f"hn2_{hb}"])
                tp = banks[0][:].bitcast(BF16).rearrange("p (a b) -> p a b", a=8)
                for kc in range(8):
                    S.add("pe", lambda e, kc=kc, hb=hb, tp=tp: e.transpose(out=tp[:, kc, :], in_=hn2[hb][:, kc * 128:(kc + 1) * 128],
                                                                           identity=ident),
                          reads=[f"hn2_{hb}", "ident"], writes=["bank0"])
                S.add("dve", lambda e, tp=tp, s=s: e.tensor_tensor(out=h2T[:, :, s * 128:(s + 1) * 128], in0=tp, in1=g2bc, op=ALU.mult),
                      reads=["bank0", "g2bc"], writes=["h2T"])
            lgp = banks[1][:, 0:80].rearrange("p (s n) -> p s n", s=4)
            for s in range(4):
                for kc in range(8):
                    S.add("pe", lambda e, s=s, kc=kc: e.matmul(lgp[:, s, :], lhsT=h2T[:, kc, s * 128:(s + 1) * 128], rhs=rw[:, kc, :],
                                                               start=(kc == 0), stop=False),
                          reads=["h2T", "rw_a", "rw_b"], writes=["bank1"])
                S.add("pe", lambda e, s=s: e.matmul(lgp[:, s, :], lhsT=ones_bf[0:1, :], rhs=rb_bf[0:1, 0:20], start=False, stop=True),
                      reads=["ones_bf", "rb_bf"], writes=["bank1"])
            S.add("dve", lambda e: e.tensor_copy(out=lg, in_=lgp), reads=["bank1"], writes=["lg"])
            GL = lg[:, :, 0:4]
            EL = lg[:, :, 4:20]
            S.add("dve", lambda e: e.tensor_reduce(out=gmax, in_=GL, axis=AX.X, op=ALU.max), reads=["lg"], writes=["gmax"])
            gmax_bc = gmax.unsqueeze(2).to_broadcast([128, 4, 4])
            S.add("dve", lambda e: e.tensor_tensor(out=gsh, in0=GL, in1=gmax_bc, op=ALU.subtract), reads=["lg", "gmax"], writes=["gsh"])
            S.add("dve", lambda e: e.tensor_tensor(out=gmask, in0=GL, in1=gmax_bc, op=ALU.is_equal), reads=["lg", "gmax"], writes=["gmask"])
            S.add("act", lambda e: e.activation(out=gsh, in_=gsh, func=AF.Exp), reads=["gsh"], writes=["gsh"])
            S.add("dve", lambda e: e.tensor_reduce(out=gsum, in_=gsh, axis=AX.X, op=ALU.add), reads=["gsh"], writes=["gsum"])
            S.add("dve", lambda e: e.reciprocal(out=ptop, in_=gsum), reads=["gsum"], writes=["ptop"])
            S.add("dve", lambda e: e.tensor_scalar(out=gmask, in0=gmask, scalar1=-1.0, scalar2=1e30, op0=ALU.add, op1=ALU.mult),
                  reads=["gmask"], writes=["gmask"])
            for s in range(4):
                S.add("dve", lambda e, s=s: e.tensor_tensor(
                    out=elm[:, s, :].rearrange("p (g k) -> p g k", g=4), in0=EL[:, s, :].rearrange("p (g k) -> p g k", g=4),
                    in1=gmask[:, s, :].unsqueeze(2).to_broadcast([128, 4, 4]), op=ALU.add),
                    reads=["lg", "gmask"], writes=["elm"])
            for s in range(4):
                S.add("dve", lambda e, s=s: e.max(out=mx8[:, s, :], in_=elm[:, s, :]), reads=["elm"], writes=["mx8"])
            S.add("dve", lambda e: e.tensor_tensor(out=dd, in0=mx8[:, :, 1], in1=mx8[:, :, 0], op=ALU.subtract), reads=["mx8"], writes=["dd"])
            S.add("act", lambda e: e.activation(out=dd, in_=dd, func=AF.Exp), reads=["dd"], writes=["dd"])
            S.add("dve", lambda e: e.tensor_scalar(out=dd, in0=dd, scalar1=1.0, scalar2=None, op0=ALU.add), reads=["dd"], writes=["dd"])
            S.add("dve", lambda e: e.reciprocal(out=dd, in_=dd), reads=["dd"], writes=["dd"])
            S.add("dve", lambda e: e.tensor_tensor(out=w1, in0=ptop, in1=dd, op=ALU.mult), reads=["ptop", "dd"], writes=["w1"])
            S.add("dve", lambda e: e.tensor_tensor(out=w2, in0=ptop, in1=w1, op=ALU.subtract), reads=["ptop", "w1"], writes=["w2"])
            S.add("dve", lambda e: e.tensor_tensor(out=eq1, in0=elm, in1=mx8[:, :, 0:1].to_broadcast([128, 4, 16]), op=ALU.is_equal),
                  reads=["elm", "mx8"], writes=["eq1"])
            S.add("dve", lambda e: e.tensor_tensor(out=eq2, in0=elm, in1=mx8[:, :, 1:2].to_broadcast([128, 4, 16]), op=ALU.is_equal),
                  reads=["elm", "mx8"], writes=["eq2"])
            S.add("dve", lambda e: e.tensor_tensor(out=eq1, in0=eq1, in1=w1.unsqueeze(2).to_broadcast([128, 4, 16]), op=ALU.mult),
                  reads=["eq1", "w1"], writes=["eq1"])
            S.add("dve", lambda e: e.tensor_tensor(out=eq2, in0=eq2, in1=w2.unsqueeze(2).to_broadcast([128, 4, 16]), op=ALU.mult),
                  reads=["eq2", "w2"], writes=["eq2"])
            S.add("dve", lambda e: e.tensor_tensor(out=comb, in0=eq1, in1=eq2, op=ALU.add), reads=["eq1", "eq2"], writes=["comb"])
            for ex in range(NE):
                eb = ectr[0] % 2
                ectr[0] += 1
                S.add("sp", lambda e, eb=eb, ex=ex: e.dma_start(out=wgb[eb], in_=wg_bf[ex]), reads=["wg_bf"], writes=[f"wgb{eb}"], tag=f"wgb{eb}")
                S.add("sp", lambda e, eb=eb, ex=ex: e.dma_start(out=wub[eb], in_=wu_bf[ex]), reads=["wu_bf"], writes=[f"wub{eb}"], tag=f"wub{eb}")
                S.add("sp", lambda e, eb=eb, ex=ex: e.dma_start(out=wdb[eb], in_=wd_bf[ex]), reads=["wd_bf"], writes=[f"wdb{eb}"], tag=f"wdb{eb}")
                for hc in range(4):
                    pg, pgn = next_gu()
                    pu, pun = next_gu()
                    for kc in range(8):
                        S.add("pe", lambda e, pg=pg, kc=kc, hc=hc, eb=eb: e.matmul(pg, lhsT=wgb[eb][:, kc, hc * 128:(hc + 1) * 128], rhs=h2T[:, kc, :],
                                                                                  start=(kc == 0), stop=(kc == 7)),
                              reads=[f"wgb{eb}", "h2T"], writes=[pgn])
                    for kc in range(8):
                        S.add("pe", lambda e, pu=pu, kc=kc, hc=hc, eb=eb: e.matmul(pu, lhsT=wub[eb][:, kc, hc * 128:(hc + 1) * 128], rhs=h2T[:, kc, :],
                                                                                  start=(kc == 0), stop=(kc == 7)),
                              reads=[f"wub{eb}", "h2T"], writes=[pun])
                    sb = hc % 2
                    S.add("act", lambda e, pg=pg, sb=sb: e.activation(out=sg[sb], in_=pg, func=AF.Silu), reads=[pgn], writes=[f"sg{sb}"])
                    S.add("dve", lambda e, pu=pu, sb=sb, eb=eb, hc=hc: e.tensor_tensor(out=he[eb][:, hc, :], in0=sg[sb], in1=pu, op=ALU.mult),
                          reads=[pun, f"sg{sb}"], writes=[f"he{eb}_{hc}"])
                for s in range(4):
                    for half in range(2):
                        pd, pdn = next_dn()
                        for hc in range(4):
                            S.add("pe", lambda e, pd=pd, hc=hc, s=s, half=half, eb=eb: e.matmul(
                                pd, lhsT=he[eb][:, hc, s * 128:(s + 1) * 128], rhs=wdb[eb][:, hc, half * 512:(half + 1) * 512],
                                start=(hc == 0), stop=(hc == 3)),
                                reads=[f"he{eb}_{hc}", f"wdb{eb}"], writes=[pdn])
                        S.add("dve", lambda e, pd=pd, X=X, s=s, half=half, ex=ex: e.scalar_tensor_tensor(
                            out=X[:, s, half * 512:(half + 1) * 512], in0=pd, scalar=comb[:, s, ex:ex + 1],
                            in1=X[:, s, half * 512:(half + 1) * 512], op0=ALU.mult, op1=ALU.add),
                            reads=[pdn, "comb", f"x1t{xb}"], writes=[f"x1t{xb}"])
            for s in range(4):
                S.add("act", lambda e, X=X, s=s: e.activation(out=junk2, in_=X[:, s, :], func=AF.Square, accum_out=ssq3[:, s:s + 1]),
                      reads=[f"x1t{xb}"], writes=["ssq3"])
            S.add("act", lambda e: e.activation(out=std3, in_=ssq3, func=AF.Sqrt, scale=1.0 / D, bias=EPS),
                  reads=["ssq3"], writes=["std3"])
            S.add("dve", lambda e: e.reciprocal(out=rstd3, in_=std3), reads=["std3"], writes=["rstd3"])
            for s in range(4):
                S.add("dve", lambda e, X=X, s=s: e.scalar_tensor_tensor(out=X[:, s, :], in0=X[:, s, :], scalar=rstd3[:, s:s + 1], in1=gfbc,
                                                                       op0=ALU.mult, op1=ALU.mult),
                      reads=[f"x1t{xb}", "rstd3", "gfbc"], writes=[f"x1t{xb}"])
            S.add("sp", lambda e, X=X, ti=ti: e.dma_start(out=out[ti * T2:(ti + 1) * T2, :].rearrange("(s p) d -> p s d", p=128), in_=X),
                  reads=[f"x1t{xb}"], writes=[f"out{xb}"], tag=f"out{xb}")
        S.add("sp", None, reads=[f"out{b}" for b in range(NB3)])

    sem_es = ExitStack()
    es.enter_context(sem_es)
    S.emit(lambda name: sem_es.enter_context(nc.semaphore(name)))
    es.close()
    if os.environ.get("KDEBUG"):
        print("ops", S.nops, "waits", S.nwaits, "arena phase1 end", A1_end)
    return nc


_INPUT_ORDER = ["x", "norm1_g", "w_in", "pool_w", "pool_scale", "conv_w", "conv_b", "rg_w_r", "rg_b_r", "rg_w_i", "rg_b_i",
                "rg_lambda", "proj_a", "proj_b", "w_out", "norm2_g", "router_group_w", "router_group_b", "router_expert_w",
                "router_expert_b", "exp_w_gate", "exp_w_up", "exp_w_down", "norm_f_g"]


def make_in_maps(inputs, n_cores, n_seq, seq_len):
    shared = {}
    for k in _INPUT_ORDER:
        if k == "x":
            continue
        a = np.asarray(inputs[k], dtype=np.float32)
        if k != "norm_f_g":
            a = a[0]
        shared[k] = np.ascontiguousarray(a)
    xfull = np.asarray(inputs["x"], dtype=np.float32)
    in_maps = []
    for c in range(n_cores):
        m = dict(shared)
        m["x"] = np.ascontiguousarray(xfull[c * n_seq:(c + 1) * n_seq].reshape(n_seq * seq_len, D))
        in_maps.append(m)
    return in_maps


def kernel(**inputs):
    x = np.asarray(inputs["x"])
    B, SEQ, _ = x.shape
    n_seq = B // NCORES
    nc = build_nc(n_seq=n_seq, seq_len=SEQ)
    in_maps = make_in_maps(inputs, NCORES, n_seq, SEQ)
    res = run_bass_kernel_spmd(nc, in_maps, core_ids=list(range(NCORES)))
    outs = [np.asarray(r["out"]).reshape(n_seq, SEQ, D) for r in res.results]
    return np.concatenate(outs, axis=0).astype(np.float32)
```

```python
import os
from contextlib import ExitStack

import numpy as np
import concourse.bass as bass
import concourse.mybir as mybir
from concourse.bass_utils import run_bass_kernel_spmd

F32 = mybir.dt.float32
BF16 = mybir.dt.bfloat16
ALU = mybir.AluOpType
AF = mybir.ActivationFunctionType
AX = mybir.AxisListType

ENGS = ("pe", "act", "dve", "pool", "sp")

D = 1024
NCORES = 8
EPS = 1e-6


class Op:
    __slots__ = ("eng", "fn", "deps", "tag", "signal", "val", "waits")

    def __init__(self, eng, fn, tag):
        self.eng = eng
        self.fn = fn
        self.tag = tag
        self.deps = []
        self.signal = False
        self.val = None
        self.waits = []


class Sched:
    def __init__(self, nc):
        self.nc = nc
        self.q = {e: [] for e in ENGS}
        self.reg = {}
        self.nops = 0

    def add(self, eng, fn, reads=(), writes=(), tag=None):
        op = Op(eng, fn, tag)
        self.nops += 1
        deps = {}
        for r in reads:
            st = self.reg.get(r)
            if st is not None and st[0] is not None:
                deps[id(st[0])] = st[0]
        for w in writes:
            st = self.reg.get(w)
            if st is not None:
                if st[0] is not None:
                    deps[id(st[0])] = st[0]
                for o in st[1]:
                    deps[id(o)] = o
        for r in reads:
            st = self.reg.get(r)
            if st is None:
                st = [None, []]
                self.reg[r] = st
            st[1].append(op)
        for w in writes:
            self.reg[w] = [op, []]
        deps.pop(id(op), None)
        op.deps = list(deps.values())
        self.q[eng].append(op)
        return op

    def barrier(self):
        lasts = [self.q[e][-1] for e in ENGS if self.q[e]]
        last_dma = {}
        for e in ENGS:
            for o in self.q[e]:
                if o.tag is not None:
                    last_dma[o.tag] = o
        deps = lasts + list(last_dma.values())
        for e in ENGS:
            op = Op(e, None, None)
            self.nops += 1
            op.deps = [d for d in deps]
            self.q[e].append(op)
        self.reg = {}

    def emit(self, mksem):
        nc = self.nc
        for e in ENGS:
            for op in self.q[e]:
                for d in op.deps:
                    if d.tag is None and d.eng == op.eng and op.eng == "pe":
                        continue
                    if d.fn is None:
                        continue
                    d.signal = True
        cnt = {}
        for e in ENGS:
            for op in self.q[e]:
                if op.fn is None:
                    continue
                if op.tag is not None:
                    key = ("dma", op.tag)
                    cnt[key] = cnt.get(key, 0) + 16
                    op.val = (key, cnt[key])
                    op.signal = True
                elif op.signal:
                    key = ("eng", e)
                    cnt[key] = cnt.get(key, 0) + 1
                    op.val = (key, cnt[key])
        sems = {key: mksem("s_" + "_".join(str(k) for k in key)) for key in cnt}
        nwaits = 0
        for e in ENGS:
            known = {}
            for op in self.q[e]:
                need = {}
                for d in op.deps:
                    if d.fn is None:
                        continue
                    if d.tag is None and d.eng == op.eng and op.eng == "pe":
                        continue
                    key, v = d.val
                    if known.get(key, 0) >= v:
                        continue
                    if need.get(key, 0) < v:
                        need[key] = v
                for key, v in need.items():
                    known[key] = v
                op.waits = list(need.items())
                nwaits += len(op.waits)
        self.nwaits = nwaits
        engobj = {"pe": "tensor", "act": "scalar", "dve": "vector", "pool": "gpsimd", "sp": "sync"}
        with nc.Block() as block:
            for e in ENGS:
                ops = self.q[e]
                if not ops:
                    continue

                def body(eng, ops=ops):
                    for op in ops:
                        for key, v in op.waits:
                            eng.wait_ge(sems[key], v)
                        if op.fn is None:
                            continue
                        ins = op.fn(eng)
                        if op.signal:
                            key, v = op.val
                            ins.then_inc(sems[key], 16 if op.tag is not None else 1)

                getattr(block, engobj[e])(body)


class Arena:
    def __init__(self, tensor, n):
        self.t = tensor
        self.n = n
        self.off = 0

    def alloc(self, shape, dtype):
        size = 2 if dtype == BF16 else 4
        nel = int(np.prod(shape))
        nb16 = (nel * size + 1) // 2
        nb16 = (nb16 + 15) // 16 * 16
        if self.off + nb16 > self.n:
            raise RuntimeError(f"arena overflow: need {self.off + nb16} > {self.n}")
        ap = self.t[:, self.off:self.off + nb16]
        self.off += nb16
        if size == 4:
            ap = ap.bitcast(dtype)[:, 0:nel]
        else:
            ap = ap[:, 0:nel]
        if len(shape) == 2:
            ap = ap.rearrange("p (a b) -> p a b", a=shape[0])
        elif len(shape) == 3:
            ap = ap.rearrange("p (a b c) -> p a b c", a=shape[0], b=shape[1])
        return ap


def build_sparse_phase2(L):
    nc = L["nc"]; S = L["S"]; A = L["A"]; banks = L["banks"]
    T = L["T"]; NE = L["NE"]; NJ = L["NJ"]; NSLOT = L["NSLOT"]
    x1s = L["x1s"]; out = L["out"]; tokof = L["tokof"]; Ys = L["Ys"]; wall = L["wall"]
    ident = L["ident"]; ones_bf = L["ones_bf"]; rb_bf = L["rb_bf"]; rw = L["rw"]
    g2bc = L["g2bc"]; gfbc = L["gfbc"]; onesf = L["onesf"]
    I32 = mybir.dt.int32
    T2 = 512
    NT2 = T // T2
    NST = T // 128
    WROW = 12288

    S.barrier()
    A.off = L["A1_start"]
    EQ1 = A.alloc([NST, 16], F32)
    EQ2 = A.alloc([NST, 16], F32)
    W1 = A.alloc([NST], F32)
    W2 = A.alloc([NST], F32)
    pos1i = A.alloc([NST], I32)
    pos2i = A.alloc([NST], I32)
    widx = A.alloc([NJ], I32)
    x1t = [A.alloc([4, D], F32) for _ in range(3)]
    junk2 = A.alloc([D], BF16)
    hn2 = [A.alloc([D], BF16) for _ in range(4)]
    hn4 = hn2
    ssq2 = A.alloc([4], F32)
    std2 = A.alloc([4], F32)
    rstd2 = A.alloc([4], F32)
    h2T = A.alloc([8, T2], BF16)
    h2Ts = [h2T, A.alloc([8, T2], BF16)]
    lg = A.alloc([4, 20], F32)
    gmax = A.alloc([4], F32)
    gsh = A.alloc([4, 4], F32)
    gsum = A.alloc([4], F32)
    ptop = A.alloc([4], F32)
    gmask = A.alloc([4, 4], F32)
    elm = A.alloc([4, 16], F32)
    mx8 = A.alloc([4, 8], F32)
    dd = A.alloc([4], F32)
    off_2b = A.off

    def norm_transpose(X, xnames):
        for s in range(4):
            S.add("act", lambda e, s=s: e.activation(out=junk2, in_=X[:, s, :], func=AF.Square, accum_out=ssq2[:, s:s + 1]),
                  reads=list(xnames), writes=["ssq2"])
        S.add("act", lambda e: e.activation(out=std2, in_=ssq2, func=AF.Sqrt, scale=1.0 / D, bias=EPS),
              reads=["ssq2"], writes=["std2"])
        S.add("dve", lambda e: e.reciprocal(out=rstd2, in_=std2), reads=["std2"], writes=["rstd2"])
        for s in range(4):
            hb = s % 2
            S.add("dve", lambda e, s=s, hb=hb: e.tensor_scalar(out=hn2[hb], in0=X[:, s, :], scalar1=rstd2[:, s:s + 1],
                                                              scalar2=None, op0=ALU.mult),
                  reads=list(xnames) + ["rstd2"], writes=[f"hn2_{hb}"])
            tb = s % 2
            tp = banks[tb][:].bitcast(BF16).rearrange("p (a b) -> p a b", a=8)
            for kc in range(8):
                S.add("pe", lambda e, kc=kc, hb=hb, tp=tp: e.transpose(out=tp[:, kc, :], in_=hn2[hb][:, kc * 128:(kc + 1) * 128],
                                                                       identity=ident),
                      reads=[f"hn2_{hb}", "ident"], writes=[f"bank{tb}"])
            S.add("dve", lambda e, tp=tp, s=s: e.tensor_tensor(out=h2T[:, :, s * 128:(s + 1) * 128], in0=tp, in1=g2bc, op=ALU.mult),
                  reads=[f"bank{tb}", "g2bc"], writes=["h2T"])

    off_2a_tmp = A.off
    LG = A.alloc([NST, 20], F32)
    gmaxA = A.alloc([NST], F32)
    gshA = A.alloc([NST, 4], F32)
    gsumA = A.alloc([NST], F32)
    ptopA = A.alloc([NST], F32)
    gmaskA = A.alloc([NST, 4], F32)
    elmA = A.alloc([NST, 16], F32)
    mx8A = A.alloc([NST, 8], F32)
    ddA = A.alloc([NST], F32)
    def a_load(ti):
        xb = ti % 3
        S.add("sp", lambda e, xb=xb, ti=ti: e.dma_start(out=x1t[xb], in_=x1s[ti * T2:(ti + 1) * T2, :].rearrange("(s p) d -> p s d", p=128)),
              writes=[f"x1t{xb}"], tag=f"x1t{xb}")

    def a_norm(ti):
        xb = ti % 3
        X = x1t[xb]
        for s in range(4):
            S.add("act", lambda e, s=s, X=X: e.activation(out=junk2, in_=X[:, s, :], func=AF.Square, accum_out=ssq2[:, s:s + 1]),
                  reads=[f"x1t{xb}"], writes=["ssq2"])
        S.add("act", lambda e: e.activation(out=std2, in_=ssq2, func=AF.Sqrt, scale=1.0 / D, bias=EPS),
              reads=["ssq2"], writes=["std2"])
        S.add("dve", lambda e: e.reciprocal(out=rstd2, in_=std2), reads=["std2"], writes=["rstd2"])
        for s in range(4):
            S.add("dve", lambda e, s=s, X=X: e.tensor_scalar(out=hn4[s], in0=X[:, s, :], scalar1=rstd2[:, s:s + 1],
                                                            scalar2=None, op0=ALU.mult),
                  reads=[f"x1t{xb}", "rstd2"], writes=[f"hn4_{s}"])

    def a_T(ti):
        hb = ti % 2
        for s in range(4):
            tb = s % 2
            tp = banks[tb][:].bitcast(BF16).rearrange("p (a b) -> p a b", a=8)
            for kc in range(8):
                S.add("pe", lambda e, kc=kc, s=s, tp=tp: e.transpose(out=tp[:, kc, :], in_=hn4[s][:, kc * 128:(kc + 1) * 128], identity=ident),
                      reads=[f"hn4_{s}"], writes=[f"bank{tb}"])
            S.add("dve", lambda e, tp=tp, s=s, hb=hb: e.tensor_tensor(out=h2Ts[hb][:, :, s * 128:(s + 1) * 128], in0=tp, in1=g2bc, op=ALU.mult),
                  reads=[f"bank{tb}"], writes=[f"h2T{hb}"])

    def a_logits(ti):
        hb = ti % 2
        lb = 2 + ti % 2
        lgp = banks[lb][:, 0:80].rearrange("p (s n) -> p s n", s=4)
        for s in range(4):
            for kc in range(8):
                S.add("pe", lambda e, s=s, kc=kc, lgp=lgp, hb=hb: e.matmul(lgp[:, s, :], lhsT=h2Ts[hb][:, kc, s * 128:(s + 1) * 128], rhs=rw[:, kc, :],
                                                                           start=(kc == 0), stop=False),
                      reads=[f"h2T{hb}"], writes=[f"bank{lb}"])
            S.add("pe", lambda e, s=s, lgp=lgp: e.matmul(lgp[:, s, :], lhsT=ones_bf[0:1, :], rhs=rb_bf[0:1, 0:20], start=False, stop=True),
                  reads=[], writes=[f"bank{lb}"])
        S.add("act", lambda e, lgp=lgp, ti=ti: e.activation(out=LG[:, ti * 4:(ti + 1) * 4, :], in_=lgp, func=AF.Copy),
              reads=[f"bank{lb}"], writes=["LG"])

    a_load(0)
    if NT2 > 1:
        a_load(1)
    a_norm(0)
    a_T(0)
    for ti in range(NT2):
        if ti + 2 < NT2:
            a_load(ti + 2)
        if ti + 1 < NT2:
            a_norm(ti + 1)
        a_logits(ti)
        if ti + 1 < NT2:
            a_T(ti + 1)
    GL = LG[:, :, 0:4]
    EL = LG[:, :, 4:20]
    S.add("dve", lambda e: e.tensor_reduce(out=gmaxA, in_=GL, axis=AX.X, op=ALU.max), reads=["LG"], writes=["gmax"])
    gmax_bc = gmaxA.unsqueeze(2).to_broadcast([128, NST, 4])
    S.add("dve", lambda e: e.tensor_tensor(out=gshA, in0=GL, in1=gmax_bc, op=ALU.subtract), reads=["LG", "gmax"], writes=["gsh"])
    S.add("dve", lambda e: e.tensor_tensor(out=gmaskA, in0=GL, in1=gmax_bc, op=ALU.is_equal), reads=["LG", "gmax"], writes=["gmask"])
    S.add("act", lambda e: e.activation(out=gshA, in_=gshA, func=AF.Exp), reads=["gsh"], writes=["gsh"])
    S.add("dve", lambda e: e.tensor_reduce(out=gsumA, in_=gshA, axis=AX.X, op=ALU.add), reads=["gsh"], writes=["gsum"])
    S.add("dve", lambda e: e.reciprocal(out=ptopA, in_=gsumA), reads=["gsum"], writes=["ptop"])
    S.add("dve", lambda e: e.tensor_scalar(out=gmaskA, in0=gmaskA, scalar1=-1.0, scalar2=1e30, op0=ALU.add, op1=ALU.mult),
          reads=["gmask"], writes=["gmask"])
    for g in range(4):
        S.add("dve", lambda e, g=g: e.tensor_tensor(out=elmA[:, :, 4 * g:4 * g + 4], in0=EL[:, :, 4 * g:4 * g + 4],
                                                    in1=gmaskA[:, :, g:g + 1].to_broadcast([128, NST, 4]), op=ALU.add),
              reads=["LG", "gmask"], writes=[f"elm{g}"])
    for st in range(NST):
        S.add("dve", lambda e, st=st: e.max(out=mx8A[:, st, :], in_=elmA[:, st, :]), reads=[f"elm{g}" for g in range(4)], writes=[f"mx8_{st % 4}"])
    mxr = [f"mx8_{k}" for k in range(4)]
    elr = [f"elm{g}" for g in range(4)]
    S.add("dve", lambda e: e.tensor_tensor(out=ddA, in0=mx8A[:, :, 1], in1=mx8A[:, :, 0], op=ALU.subtract), reads=mxr, writes=["dd"])
    S.add("act", lambda e: e.activation(out=ddA, in_=ddA, func=AF.Exp), reads=["dd"], writes=["dd"])
    S.add("dve", lambda e: e.tensor_scalar(out=ddA, in0=ddA, scalar1=1.0, scalar2=None, op0=ALU.add), reads=["dd"], writes=["dd"])
    S.add("dve", lambda e: e.reciprocal(out=ddA, in_=ddA), reads=["dd"], writes=["dd"])
    S.add("dve", lambda e: e.tensor_tensor(out=W1, in0=ptopA, in1=ddA, op=ALU.mult), reads=["ptop", "dd"], writes=["W1"])
    S.add("dve", lambda e: e.tensor_tensor(out=W2, in0=ptopA, in1=W1, op=ALU.subtract), reads=["ptop", "W1"], writes=["W2"])
    S.add("dve", lambda e: e.tensor_tensor(out=EQ1, in0=elmA, in1=mx8A[:, :, 0:1].to_broadcast([128, NST, 16]), op=ALU.is_equal),
          reads=elr + mxr, writes=["EQ1"])
    S.add("dve", lambda e: e.tensor_tensor(out=EQ2, in0=elmA, in1=mx8A[:, :, 1:2].to_broadcast([128, NST, 16]), op=ALU.is_equal),
          reads=elr + mxr, writes=["EQ2"])

    NF = NST * 16
    Mall = A.alloc([NF], BF16)
    Lmat = A.alloc([128], BF16)
    Lf = A.alloc([128], F32)
    onesm = A.alloc([128], BF16)
    within = A.alloc([NST, 16], F32)
    tot = A.alloc([NST, 16], F32)
    incl = A.alloc([NST, 16], F32)
    ones64 = A.alloc([max(NST, 16)], F32)
    q = A.alloc([16], F32)
    qi = A.alloc([16], I32)
    qf = A.alloc([16], F32)
    qg = A.alloc([16], F32)
    endc = A.alloc([16], F32)
    base = A.alloc([16], F32)
    tmp3 = A.alloc([NST, 16], F32)
    posf = A.alloc([NST], F32)
    jlim = A.alloc([NJ], F32)
    cmpj = A.alloc([NJ, 16], F32)
    ejf = A.alloc([NJ], F32)
    iop = A.alloc([1], F32)
    NZ = NSLOT // 128
    zt = A.alloc([NZ, 8], I32)
    tokid = A.alloc([NST, 8], I32)

    S.add("dve", lambda e: e.tensor_tensor(out=Mall, in0=EQ1.rearrange("p a b -> p (a b)"), in1=EQ2.rearrange("p a b -> p (a b)"), op=ALU.add),
          reads=["EQ1", "EQ2"], writes=["Mall"])
    S.add("pool", lambda e: e.affine_select(out=Lf, in_=onesf, pattern=[[1, 128]], compare_op=ALU.is_gt, fill=0.0, base=0, channel_multiplier=-1),
          reads=[], writes=["Lf"])
    S.add("dve", lambda e: e.tensor_copy(out=Lmat, in_=Lf), reads=["Lf"], writes=["Lmat"])
    S.add("dve", lambda e: e.tensor_copy(out=onesm, in_=onesf), reads=[], writes=["onesm"])
    S.add("dve", lambda e: e.memset(ones64, 1.0), writes=["ones64"])
    S.add("dve", lambda e: e.memset(zt, 0), writes=["zt"])
    S.add("pool", lambda e: e.iota(tokid, pattern=[[128, NST], [0, 8]], base=0, channel_multiplier=1), writes=["tokid"])
    S.add("pool", lambda e: e.iota(jlim, pattern=[[512, NJ]], base=0, channel_multiplier=0, allow_small_or_imprecise_dtypes=True), writes=["jlim"])
    S.add("pool", lambda e: e.iota(iop, pattern=[[0, 1]], base=0, channel_multiplier=1, allow_small_or_imprecise_dtypes=True), writes=["iop"])
    S.add("sp", lambda e: e.dma_start(out=tokof.rearrange("(a p) c -> p a c", p=128), in_=zt), reads=["zt"], writes=["tokof"], tag="tokz")
    wf = within.rearrange("p a b -> p (a b)")
    tf = tot.rearrange("p a b -> p (a b)")
    for (lhs, lhsn, dst, dstn) in ((Lmat, "Lmat", wf, "within"), (onesm, "onesm", tf, "tot")):
        for h in range((NF + 511) // 512):
            bk = 3 + h % 2
            lo = h * 512
            hi = min(NF, lo + 512)
            S.add("pe", lambda e, lhs=lhs, lo=lo, hi=hi, bk=bk: e.matmul(banks[bk][:, 0:hi - lo], lhsT=lhs, rhs=Mall[:, lo:hi], start=True, stop=True),
                  reads=[lhsn, "Mall"], writes=[f"bank{bk}"])
            S.add("dve", lambda e, dst=dst, lo=lo, hi=hi, bk=bk: e.tensor_copy(out=dst[:, lo:hi], in_=banks[bk][:, 0:hi - lo]),
                  reads=[f"bank{bk}"], writes=[dstn])
    for ex in range(16):
        S.add("dve", lambda e, ex=ex: e.tensor_tensor_scan(out=incl[:, :, ex], data0=ones64[:, 0:NST], data1=tot[:, :, ex], initial=0.0,
                                                           op0=ALU.mult, op1=ALU.add),
              reads=["tot", "ones64"], writes=["incl"])
    cnt = incl[:, NST - 1, :]
    S.add("dve", lambda e: e.tensor_scalar(out=q, in0=cnt, scalar1=511.0, scalar2=1.0 / 512, op0=ALU.add, op1=ALU.mult), reads=["incl"], writes=["q"])
    S.add("dve", lambda e: e.tensor_copy(out=qi, in_=q), reads=["q"], writes=["qi"])
    S.add("dve", lambda e: e.tensor_copy(out=qf, in_=qi), reads=["qi"], writes=["qf"])
    S.add("dve", lambda e: e.tensor_tensor(out=qg, in0=qf, in1=q, op=ALU.is_gt), reads=["qf", "q"], writes=["qg"])
    S.add("dve", lambda e: e.tensor_tensor(out=qf, in0=qf, in1=qg, op=ALU.subtract), reads=["qf", "qg"], writes=["qf"])
    S.add("dve", lambda e: e.tensor_scalar(out=qf, in0=qf, scalar1=512.0, scalar2=None, op0=ALU.mult), reads=["qf"], writes=["qf"])
    S.add("dve", lambda e: e.tensor_tensor_scan(out=endc, data0=ones64[:, 0:16], data1=qf, initial=0.0, op0=ALU.mult, op1=ALU.add),
          reads=["qf", "ones64"], writes=["endc"])
    S.add("dve", lambda e: e.tensor_tensor(out=base, in0=endc, in1=qf, op=ALU.subtract), reads=["endc", "qf"], writes=["base"])
    S.add("dve", lambda e: e.tensor_tensor(out=incl, in0=incl, in1=tot, op=ALU.subtract), reads=["incl", "tot", "q"], writes=["incl"])
    S.add("dve", lambda e: e.tensor_tensor(out=within, in0=within, in1=incl, op=ALU.add), reads=["within", "incl"], writes=["within"])
    S.add("dve", lambda e: e.tensor_tensor(out=within, in0=within, in1=base.unsqueeze(1).to_broadcast([128, NST, 16]), op=ALU.add),
          reads=["within", "base"], writes=["within"])
    for (EQ, eqn, posi, posn) in ((EQ1, "EQ1", pos1i, "pos1i"), (EQ2, "EQ2", pos2i, "pos2i")):
        S.add("dve", lambda e, EQ=EQ: e.tensor_tensor(out=tmp3, in0=EQ, in1=within, op=ALU.mult), reads=[eqn, "within"], writes=["tmp3"])
        S.add("dve", lambda e: e.tensor_reduce(out=posf, in_=tmp3, axis=AX.X, op=ALU.add), reads=["tmp3"], writes=["posf"])
        S.add("dve", lambda e, posi=posi: e.tensor_copy(out=posi, in_=posf), reads=["posf"], writes=[posn])
    S.add("dve", lambda e: e.tensor_tensor(out=cmpj, in0=endc.unsqueeze(1).to_broadcast([128, NJ, 16]),
                                           in1=jlim.unsqueeze(2).to_broadcast([128, NJ, 16]), op=ALU.is_le),
          reads=["endc", "jlim"], writes=["cmpj"])
    S.add("dve", lambda e: e.tensor_reduce(out=ejf, in_=cmpj, axis=AX.X, op=ALU.add), reads=["cmpj"], writes=["ejf"])
    S.add("dve", lambda e: e.tensor_scalar(out=ejf, in0=ejf, scalar1=15.0, scalar2=128.0, op0=ALU.min, op1=ALU.mult), reads=["ejf"], writes=["ejf"])
    S.add("dve", lambda e: e.tensor_scalar(out=ejf, in0=ejf, scalar1=iop[:, 0:1], scalar2=None, op0=ALU.add), reads=["ejf", "iop"], writes=["ejf"])
    S.add("dve", lambda e: e.tensor_copy(out=widx, in_=ejf), reads=["ejf"], writes=["widx"])
    sc_regions = []
    for st in range(NST):
        for (posi, posn) in ((pos1i, "pos1i"), (pos2i, "pos2i")):
            rn = f"tsc{st}{posn}"
            sc_regions.append(rn)
            S.add("pool", lambda e, st=st, posi=posi: e.indirect_dma_start(
                out=tokof, out_offset=bass.IndirectOffsetOnAxis(ap=posi[:, st:st + 1], axis=0), in_=tokid[:, st, :], in_offset=None),
                reads=[posn, "tokid", "tokof"], writes=[rn], tag="tsc")

    A.off = off_2a_tmp
    off_2d = A.off
    wbuf = [A.alloc([WROW], BF16) for _ in range(2)]
    Yt = [A.alloc([4, D], F32) for _ in range(2)]
    sg = [A.alloc([T2], BF16) for _ in range(2)]
    he = [A.alloc([4, T2], BF16) for _ in range(2)]
    tk = [A.alloc([4, 8], I32) for _ in range(3)]
    gu_ctr = [0]

    def next_gu():
        b = (2, 3, 4, 5)[gu_ctr[0] % 4]
        gu_ctr[0] += 1
        return banks[b][:, :], f"bank{b}"

    dn_ctr = [0]

    def next_dn():
        b = (6, 7, 4, 5)[dn_ctr[0] % 4]
        dn_ctr[0] += 1
        return banks[b][:, :], f"bank{b}"

    ev_ctr = [0]
    S.barrier()

    def loads_x(j):
        b = j % 3
        X = x1t[b]
        S.add("sp", lambda e, b=b, j=j: e.dma_start(out=tk[b], in_=tokof[j * 512:(j + 1) * 512, :].rearrange("(s p) c -> p s c", p=128)),
              writes=[f"tk{b}"], tag=f"tk{b}")
        for s in range(4):
            rn = f"xg{b}_{s}"
            S.add("pool", lambda e, b=b, s=s, X=X: e.indirect_dma_start(
                out=X[:, s, :], out_offset=None, in_=x1s, in_offset=bass.IndirectOffsetOnAxis(ap=tk[b][:, s, 0:1], axis=0)),
                reads=[f"tk{b}"], writes=[rn], tag=rn)

    def loads_w(j):
        b = j % 2
        S.add("pool", lambda e, b=b, j=j: e.indirect_dma_start(
            out=wbuf[b], out_offset=None, in_=wall, in_offset=bass.IndirectOffsetOnAxis(ap=widx[:, j:j + 1], axis=0)),
            reads=["widx"], writes=[f"wbuf{b}"], tag=f"wbuf{b}")

    def normA(j):
        b = j % 3
        X = x1t[b]
        xn = [f"xg{b}_{s}" for s in range(4)]
        for s in range(4):
            S.add("act", lambda e, s=s, X=X: e.activation(out=junk2, in_=X[:, s, :], func=AF.Square, accum_out=ssq2[:, s:s + 1]),
                  reads=[xn[s]], writes=["ssq2"])
        S.add("act", lambda e: e.activation(out=std2, in_=ssq2, func=AF.Sqrt, scale=1.0 / D, bias=EPS),
              reads=["ssq2"], writes=["std2"])
        S.add("dve", lambda e: e.reciprocal(out=rstd2, in_=std2), reads=["std2"], writes=["rstd2"])
        for s in range(4):
            S.add("dve", lambda e, s=s, X=X: e.tensor_scalar(out=hn4[s], in0=X[:, s, :], scalar1=rstd2[:, s:s + 1],
                                                            scalar2=None, op0=ALU.mult),
                  reads=[xn[s], "rstd2"], writes=[f"hn4_{s}"])

    def transT(j):
        hb = j % 2
        for s in range(4):
            tb = s % 2
            tp = banks[tb][:].bitcast(BF16).rearrange("p (a b) -> p a b", a=8)
            for kc in range(8):
                S.add("pe", lambda e, kc=kc, s=s, tp=tp: e.transpose(out=tp[:, kc, :], in_=hn4[s][:, kc * 128:(kc + 1) * 128], identity=ident),
                      reads=[f"hn4_{s}"], writes=[f"bank{tb}"])
            S.add("dve", lambda e, tp=tp, s=s, hb=hb: e.tensor_tensor(out=h2Ts[hb][:, :, s * 128:(s + 1) * 128], in0=tp, in1=g2bc, op=ALU.mult),
                  reads=[f"bank{tb}"], writes=[f"h2T{hb}"])

    def gu(j):
        b = j % 2
        h2T = h2Ts[j % 2]
        h2n = f"h2T{j % 2}"
        wgv = wbuf[b][:, 0:4096].rearrange("p (k n) -> p k n", k=8)
        wuv = wbuf[b][:, 4096:8192].rearrange("p (k n) -> p k n", k=8)
        for hc in range(4):
            pg, pgn = next_gu()
            pu, pun = next_gu()
            for kc in range(8):
                S.add("pe", lambda e, pg=pg, kc=kc, hc=hc, wgv=wgv, h2T=h2T: e.matmul(pg, lhsT=wgv[:, kc, hc * 128:(hc + 1) * 128], rhs=h2T[:, kc, :],
                                                                            start=(kc == 0), stop=(kc == 7)),
                      reads=[f"wbuf{b}", h2n], writes=[pgn])
            for kc in range(8):
                S.add("pe", lambda e, pu=pu, kc=kc, hc=hc, wuv=wuv, h2T=h2T: e.matmul(pu, lhsT=wuv[:, kc, hc * 128:(hc + 1) * 128], rhs=h2T[:, kc, :],
                                                                            start=(kc == 0), stop=(kc == 7)),
                      reads=[f"wbuf{b}", h2n], writes=[pun])
            sb = hc % 2
            S.add("act", lambda e, pg=pg, sb=sb: e.activation(out=sg[sb], in_=pg, func=AF.Silu), reads=[pgn], writes=[f"sg{sb}"])
            S.add("dve", lambda e, pu=pu, sb=sb, b=b, hc=hc: e.tensor_tensor(out=he[b][:, hc, :], in0=sg[sb], in1=pu, op=ALU.mult),
                  reads=[pun, f"sg{sb}"], writes=[f"he{b}_{hc}"])

    def down(j):
        b = j % 2
        wdv = wbuf[b][:, 8192:12288].rearrange("p (k n) -> p k n", k=4)
        for s in range(4):
            for half in range(2):
                pd, pdn = next_dn()
                for hc in range(4):
                    S.add("pe", lambda e, pd=pd, hc=hc, s=s, b=b, half=half, wdv=wdv: e.matmul(
                        pd, lhsT=he[b][:, hc, s * 128:(s + 1) * 128], rhs=wdv[:, hc, half * 512:(half + 1) * 512],
                        start=(hc == 0), stop=(hc == 3)),
                        reads=[f"he{b}_{hc}", f"wbuf{b}"], writes=[pdn])
                dst = Yt[b][:, s, half * 512:(half + 1) * 512]
                if ev_ctr[0] % 2 == 0:
                    S.add("act", lambda e, pd=pd, dst=dst: e.activation(out=dst, in_=pd, func=AF.Copy), reads=[pdn], writes=[f"Yt{b}_{s}{half}"])
                else:
                    S.add("dve", lambda e, pd=pd, dst=dst: e.tensor_copy(out=dst, in_=pd), reads=[pdn], writes=[f"Yt{b}_{s}{half}"])
                ev_ctr[0] += 1
        S.add("sp", lambda e, b=b, j=j: e.dma_start(out=Ys[j * 512:(j + 1) * 512, :].rearrange("(s p) d -> p s d", p=128), in_=Yt[b]),
              reads=[f"Yt{b}_{s}{h}" for s in range(4) for h in range(2)], writes=[f"Ys{j}"], tag=f"yo{b}")

    loads_x(0)
    loads_w(0)
    if NJ > 1:
        loads_x(1)
        loads_w(1)
    normA(0)
    transT(0)
    for j in range(NJ):
        if j + 2 < NJ:
            loads_x(j + 2)
        if j + 1 < NJ:
            normA(j + 1)
        gu(j)
        if j + 1 < NJ:
            transT(j + 1)
        down(j)
        if j + 2 < NJ:
            loads_w(j + 2)

    S.barrier()
    A.off = off_2d
    NB3 = 6
    C0 = [A.alloc([D], F32) for _ in range(NB3)]
    C1 = [A.alloc([D], F32) for _ in range(NB3)]
    C2 = [A.alloc([D], F32) for _ in range(NB3)]
    ssq3 = [A.alloc([1], F32) for _ in range(NB3)]
    std3 = [A.alloc([1], F32) for _ in range(NB3)]
    rstd3 = [A.alloc([1], F32) for _ in range(NB3)]

    def loads_d(st):
        b = st % NB3
        S.add("sp", lambda e, b=b, st=st: e.dma_start(out=C0[b], in_=x1s[st * 128:(st + 1) * 128, :]), writes=[f"C0{b}"], tag=f"c0{b}")
        S.add("pool", lambda e, b=b, st=st: e.indirect_dma_start(
            out=C1[b], out_offset=None, in_=Ys, in_offset=bass.IndirectOffsetOnAxis(ap=pos1i[:, st:st + 1], axis=0)),
            writes=[f"C1{b}"], tag=f"c1{b}")
        S.add("pool", lambda e, b=b, st=st: e.indirect_dma_start(
            out=C2[b], out_offset=None, in_=Ys, in_offset=bass.IndirectOffsetOnAxis(ap=pos2i[:, st:st + 1], axis=0)),
            writes=[f"C2{b}"], tag=f"c2{b}")

    def d_ab(st):
        b = st % NB3
        S.add("dve", lambda e, b=b, st=st: e.scalar_tensor_tensor(out=C0[b], in0=C1[b], scalar=W1[:, st:st + 1], in1=C0[b], op0=ALU.mult, op1=ALU.add),
              reads=[f"C1{b}", f"C0{b}"], writes=[f"C0{b}"])
        S.add("dve", lambda e, b=b, st=st: e.scalar_tensor_tensor(out=C0[b], in0=C2[b], scalar=W2[:, st:st + 1], in1=C0[b], op0=ALU.mult, op1=ALU.add),
              reads=[f"C2{b}", f"C0{b}"], writes=[f"C0{b}"])
        S.add("act", lambda e, b=b: e.activation(out=junk2, in_=C0[b], func=AF.Square, accum_out=ssq3[b]), reads=[f"C0{b}"], writes=[f"ssq3{b}"])
        S.add("act", lambda e, b=b: e.activation(out=std3[b], in_=ssq3[b], func=AF.Sqrt, scale=1.0 / D, bias=EPS), reads=[f"ssq3{b}"], writes=[f"std3{b}"])

    def d_fin(st):
        b = st % NB3
        S.add("dve", lambda e, b=b: e.reciprocal(out=rstd3[b], in_=std3[b]), reads=[f"std3{b}"], writes=[f"rstd3{b}"])
        S.add("dve", lambda e, b=b: e.scalar_tensor_tensor(out=C0[b], in0=C0[b], scalar=rstd3[b][:, 0:1], in1=gfbc, op0=ALU.mult, op1=ALU.mult),
              reads=[f"C0{b}", f"rstd3{b}"], writes=[f"C0{b}"])
        S.add("sp", lambda e, b=b, st=st: e.dma_start(out=out[st * 128:(st + 1) * 128, :], in_=C0[b]), reads=[f"C0{b}"], writes=[f"out{b}"], tag=f"co{b}")

    for st in range(min(NB3, NST)):
        loads_d(st)
    d_ab(0)
    for st in range(NST):
        if st + 1 < NST:
            d_ab(st + 1)
        d_fin(st)
        if st + NB3 < NST:
            loads_d(st + NB3)
    S.add("sp", None, reads=[f"out{b}" for b in range(NB3)])


def build_nc(n_seq=2, seq_len=4096, stop_after_phase1=False, sparse=True):
    T = n_seq * seq_len
    T1 = 256
    NT1 = T // T1
    TPS1 = seq_len // T1
    T2 = 512
    NT2 = T // T2
    NE = 16

    nc = bass.Bass("TRN2", target_bir_lowering=False)

    def din(name, shape):
        return nc.dram_tensor(name, shape, F32, kind="ExternalInput").ap()

    x = din("x", [T, D])
    norm1_g = din("norm1_g", [D])
    w_in = din("w_in", [D, 4608])
    pool_w = din("pool_w", [4, 128, 128])
    pool_scale = din("pool_scale", [512])
    conv_w = din("conv_w", [4, D])
    conv_b = din("conv_b", [D])
    rg_w_r = din("rg_w_r", [16, 64, 64])
    rg_b_r = din("rg_b_r", [D])
    rg_w_i = din("rg_w_i", [16, 64, 64])
    rg_b_i = din("rg_b_i", [D])
    rg_lambda = din("rg_lambda", [D])
    proj_a = din("proj_a", [512, D])
    proj_b = din("proj_b", [D, D])
    w_out = din("w_out", [D, D])
    norm2_g = din("norm2_g", [D])
    router_group_w = din("router_group_w", [D, 4])
    router_group_b = din("router_group_b", [4])
    router_expert_w = din("router_expert_w", [D, 16])
    router_expert_b = din("router_expert_b", [16])
    exp_w_gate = din("exp_w_gate", [NE, D, 512])
    exp_w_up = din("exp_w_up", [NE, D, 512])
    exp_w_down = din("exp_w_down", [NE, 512, D])
    norm_f_g = din("norm_f_g", [D])
    out = nc.dram_tensor("out", [T, D], F32, kind="ExternalOutput").ap()

    w_in_bf = nc.dram_tensor("w_in_bf", [9, 128, 8, 512], BF16, kind="Internal").ap()
    WROW = 12288
    wall = nc.dram_tensor("wall", [NE * 128, WROW], BF16, kind="Internal").ap()
    wall_v = wall.rearrange("(e p) n -> e p n", p=128)

    class _WV:
        def __init__(self, lo, k):
            self.lo, self.k = lo, k

        def __getitem__(self, e):
            return wall_v[e][:, self.lo:self.lo + 4096].rearrange("p (k n) -> p k n", k=self.k)

    wg_bf = _WV(0, 8)
    wu_bf = _WV(4096, 8)
    wd_bf = _WV(8192, 4)
    NJ = (2 * T) // 512 + NE
    NSLOT = NJ * 512
    I32 = mybir.dt.int32
    tokof = nc.dram_tensor("tokof", [NSLOT, 8], I32, kind="Internal").ap()
    Ys = nc.dram_tensor("Ys", [NSLOT, D], F32, kind="Internal").ap()
    x1s = nc.dram_tensor("x1s", [T, D], F32, kind="Internal").ap()

    es = ExitStack()
    NA = 105000
    arena_t = es.enter_context(nc.sbuf_tensor("arena", [128, NA], BF16))
    banks = [es.enter_context(nc.psum_tensor(f"bank{i}", [128, 512], F32)) for i in range(8)]
    A = Arena(arena_t, NA)
    S = Sched(nc)

    def vcol(v):
        return v.rearrange("(k p) -> p k", p=128)

    ident = A.alloc([128], BF16)
    identf = A.alloc([128], F32)
    onesf = A.alloc([128], F32)
    ones_bf = A.alloc([128], BF16)
    g1T = A.alloc([8], F32)
    g2T = A.alloc([8], F32)
    g1bc = A.alloc([8, 128], BF16)
    g2bc = A.alloc([8, 128], BF16)
    psT = A.alloc([4], F32)
    cwT = A.alloc([4, 8], F32)
    cbT = A.alloc([8], F32)
    hbr = A.alloc([8], F32)
    hbi = A.alloc([8], F32)
    lamT = A.alloc([8], F32)
    chalf = A.alloc([8], F32)
    invc = A.alloc([16], F32)
    gfrow = A.alloc([D], F32)
    gfbc = A.alloc([D], F32)
    rbrow = A.alloc([32], F32)
    rb_bf = A.alloc([32], BF16)
    rw = A.alloc([8, 20], BF16)
    const_end = A.off

    def small_load(dst, src, name):
        S.add("sp", lambda e: e.dma_start(out=dst, in_=src, allow_slow_non_contiguous=True),
              writes=[name], tag=name)

    small_load(g1T, vcol(norm1_g), "g1T")
    small_load(g2T, vcol(norm2_g), "g2T")
    small_load(psT, pool_scale.rearrange("(k p) -> p k", p=128), "psT")
    small_load(cwT, conv_w.rearrange("k (c p) -> p k c", p=128), "cwT")
    small_load(cbT, vcol(conv_b), "cbT")
    small_load(hbr, vcol(rg_b_r), "hbr")
    small_load(hbi, vcol(rg_b_i), "hbi")
    small_load(lamT, vcol(rg_lambda), "lamT")
    S.add("sp", lambda e: e.dma_start(out=gfrow[0:1, :], in_=norm_f_g.rearrange("(o n) -> o n", o=1)),
          writes=["gfrow"], tag="gfrow")
    S.add("sp", lambda e: e.dma_start(out=rbrow[0:1, 0:4], in_=router_group_b.rearrange("(o n) -> o n", o=1)),
          writes=["rbrow_a"], tag="rbrow_a")
    S.add("sp", lambda e: e.dma_start(out=rbrow[0:1, 4:20], in_=router_expert_b.rearrange("(o n) -> o n", o=1)),
          writes=["rbrow_b"], tag="rbrow_b")
    S.add("pool", lambda e: e.dma_start(out=rw[:, :, 0:4], in_=router_group_w.rearrange("(k p) n -> p k n", p=128)),
          writes=["rw_a"], tag="rw_a")
    S.add("pool", lambda e: e.dma_start(out=rw[:, :, 4:20], in_=router_expert_w.rearrange("(k p) n -> p k n", p=128)),
          writes=["rw_b"], tag="rw_b")

    S.add("pool", lambda e: e.memset(onesf, 1.0), writes=["onesf"])
    S.add("pool", lambda e: e.affine_select(out=identf, in_=onesf, pattern=[[-1, 128]], compare_op=ALU.is_equal,
                                            fill=0.0, base=0, channel_multiplier=1),
          reads=["onesf"], writes=["identf"])
    S.add("dve", lambda e: e.tensor_copy(out=ident, in_=identf), reads=["identf"], writes=["ident"])
    S.add("dve", lambda e: e.tensor_copy(out=ones_bf, in_=onesf), reads=["onesf"], writes=["ones_bf"])
    S.add("dve", lambda e: e.tensor_copy(out=g1bc, in_=g1T.unsqueeze(2).to_broadcast([128, 8, 128])),
          reads=["g1T"], writes=["g1bc"])
    S.add("dve", lambda e: e.tensor_copy(out=g2bc, in_=g2T.unsqueeze(2).to_broadcast([128, 8, 128])),
          reads=["g2T"], writes=["g2bc"])
    S.add("dve", lambda e: e.tensor_copy(out=rb_bf[0:1, 0:20], in_=rbrow[0:1, 0:20]),
          reads=["rbrow_a", "rbrow_b"], writes=["rb_bf"])
    S.add("dve", lambda e: e.tensor_scalar(out=hbr, in0=hbr, scalar1=0.5, scalar2=None, op0=ALU.mult),
          reads=["hbr"], writes=["hbr"])
    S.add("dve", lambda e: e.tensor_scalar(out=hbi, in0=hbi, scalar1=0.5, scalar2=None, op0=ALU.mult),
          reads=["hbi"], writes=["hbi"])
    sp_a = A.alloc([8], F32)
    sp_b = A.alloc([8], F32)
    S.add("dve", lambda e: e.tensor_scalar(out=sp_b, in0=lamT, scalar1=-1.0, scalar2=None, op0=ALU.mult),
          reads=["lamT"], writes=["sp_b"])
    S.add("dve", lambda e: e.tensor_tensor(out=sp_a, in0=lamT, in1=sp_b, op=ALU.max),
          reads=["lamT", "sp_b"], writes=["sp_a"])
    S.add("act", lambda e: e.activation(out=sp_a, in_=sp_a, func=AF.Exp, scale=-1.0), reads=["sp_a"], writes=["sp_a"])
    S.add("act", lambda e: e.activation(out=sp_a, in_=sp_a, func=AF.Ln, bias=1.0), reads=["sp_a"], writes=["sp_a"])
    S.add("dve", lambda e: e.tensor_scalar(out=sp_b, in0=lamT, scalar1=-1.0, scalar2=0.0, op0=ALU.mult, op1=ALU.max),
          reads=["lamT"], writes=["sp_b"])
    S.add("dve", lambda e: e.tensor_tensor(out=sp_a, in0=sp_a, in1=sp_b, op=ALU.add), reads=["sp_a", "sp_b"], writes=["sp_a"])
    S.add("dve", lambda e: e.tensor_scalar(out=chalf, in0=sp_a, scalar1=-4.0, scalar2=None, op0=ALU.mult),
          reads=["sp_a"], writes=["chalf"])
    for t in range(16):
        S.add("pool", lambda e, t=t: e.memset(invc[:, t:t + 1], 1.0 / (t + 1)), writes=["invc"])
    for h in range(2):
        S.add("pe", lambda e, h=h: e.matmul(banks[h][:, :], lhsT=onesf[0:1, :], rhs=gfrow[0:1, h * 512:(h + 1) * 512],
                                            start=True, stop=True),
              reads=["onesf", "gfrow"], writes=[f"bank{h}"])
        S.add("dve", lambda e, h=h: e.tensor_copy(out=gfbc[:, h * 512:(h + 1) * 512], in_=banks[h][:, :]),
              reads=[f"bank{h}"], writes=["gfbc"])

    for g in range(9):
        S.add("pool", lambda e, g=g: e.dma_start(out=w_in_bf[g], in_=w_in[:, g * 512:(g + 1) * 512].rearrange("(k p) n -> p k n", p=128)),
              writes=[f"w_in_bf{g}"], tag=f"w_in_bf{g}")

    A1_start = A.off
    pa_w = A.alloc([4, D], BF16)
    pb_w = A.alloc([8, D], BF16)
    wo_w = A.alloc([8, D], BF16)
    pl_w = A.alloc([4, 128], BF16)
    wr_bd = A.alloc([8, 128], BF16)
    wi_bd = A.alloc([8, 128], BF16)
    dg = A.alloc([4, 8, 128], BF16)
    S.add("pool", lambda e: e.dma_start(out=pl_w, in_=pool_w.rearrange("g c d -> c g d")), writes=["pl_w"], tag="pl_w")
    S.add("pool", lambda e: e.dma_start(out=pa_w, in_=proj_a.rearrange("(k p) n -> p k n", p=128)), writes=["pa_w"], tag="pa_w")
    S.add("dve", lambda e: e.memset(wr_bd, 0.0), writes=["wr_bd"])
    S.add("dve", lambda e: e.memset(wi_bd, 0.0), writes=["wi_bd"])
    for (wsrc, wdst, nm) in ((rg_w_r, wr_bd, "wr_bd"), (rg_w_i, wi_bd, "wi_bd")):
        for hh in range(2):
            src = wsrc.rearrange("(c two) a b -> two a c b", two=2)[hh]
            S.add("pool", lambda e, src=src, wdst=wdst, hh=hh: e.dma_start(
                out=wdst[hh * 64:(hh + 1) * 64, :, hh * 64:(hh + 1) * 64], in_=src),
                writes=[nm], tag=nm + str(hh))
    S.add("pool", lambda e: e.dma_start(out=pb_w, in_=proj_b.rearrange("(k p) n -> p k n", p=128)), writes=["pb_w"], tag="pb_w")
    S.add("pool", lambda e: e.dma_start(out=wo_w, in_=w_out.rearrange("(k p) n -> p k n", p=128)), writes=["wo_w"], tag="wo_w")
    for k in range(4):
        for c in range(8):
            S.add("dve", lambda e, k=k, c=c: e.tensor_scalar(out=dg[:, k, c, :], in0=identf, scalar1=cwT[:, k, c:c + 1],
                                                              scalar2=None, op0=ALU.mult),
                  reads=["identf", "cwT"], writes=["dg"])
    def cast_expert(e_, after):
        S.add("pool", lambda e, e_=e_: e.dma_start(out=wg_bf[e_], in_=exp_w_gate[e_].rearrange("(k p) n -> p k n", p=128)),
              reads=after, writes=[f"wg_bf{e_}"], tag="wg_bf")
        S.add("pool", lambda e, e_=e_: e.dma_start(out=wu_bf[e_], in_=exp_w_up[e_].rearrange("(k p) n -> p k n", p=128)),
              reads=after, writes=[f"wu_bf{e_}"], tag="wu_bf")
        S.add("pool", lambda e, e_=e_: e.dma_start(out=wd_bf[e_], in_=exp_w_down[e_].rearrange("(k p) n -> p k n", p=128)),
              reads=after, writes=[f"wd_bf{e_}"], tag="wd_bf")

    cast_state = [0]

    NXS = 6
    xs = [A.alloc([D], F32) for _ in range(NXS)]
    hn = [A.alloc([D], BF16) for _ in range(2)]
    ssq = [A.alloc([2], F32) for _ in range(2)]
    std = [A.alloc([2], F32) for _ in range(2)]
    rstd = [A.alloc([2], F32) for _ in range(2)]
    hT = [A.alloc([8, T1], BF16) for _ in range(2)]
    wb = [A.alloc([8, 512], BF16) for _ in range(3)]
    UPW = 16 + T1
    up = A.alloc([4, UPW], F32)
    s1 = A.alloc([UPW], F32)
    s2 = A.alloc([UPW], F32)
    ptmp = A.alloc([16], F32)
    z = A.alloc([4, T1], BF16)
    pa = A.alloc([4, T1], BF16)
    ULW = 3 + T1
    ul = A.alloc([8, ULW], BF16)
    gg2 = [A.alloc([8, T1], BF16) for _ in range(2)]
    tha2 = [A.alloc([8, T1], BF16) for _ in range(2)]
    thb2 = [A.alloc([8, T1], BF16) for _ in range(2)]
    xc = [A.alloc([T1], BF16) for _ in range(2)]
    thr = [A.alloc([T1], F32) for _ in range(2)]
    thi = [A.alloc([T1], F32) for _ in range(2)]
    a_t = A.alloc([8, T1], F32)
    t_t = A.alloc([8, T1], F32)
    mult = [A.alloc([T1], F32) for _ in range(2)]
    hs = [A.alloc([T1], F32) for _ in range(2)]
    hst = A.alloc([8], F32)
    hg = A.alloc([8, T1], BF16)
    m1 = A.alloc([8, T1], BF16)
    m2 = A.alloc([8, T1], BF16)
    A1_end = A.off

    slotA = [(b, 0) for b in (2, 3, 4, 5)]
    slot_ctr = [0]

    def next_slot():
        b, h = slotA[slot_ctr[0] % len(slotA)]
        slot_ctr[0] += 1
        return banks[b][:, h * 256:(h + 1) * 256], f"bank{b}"

    bigB = [6, 7]
    big_ctr = [0]

    def next_big():
        b = bigB[big_ctr[0] % 2]
        big_ctr[0] += 1
        return banks[b][:, :], f"bank{b}"

    x_t = x.rearrange("(n p) d -> n p d", p=128)
    x1_t = x1s.rearrange("(n p) d -> n p d", p=128)
    out_t = out.rearrange("(n p) d -> n p d", p=128)
    sub_ctr = [0]
    wb_ctr = [0]

    xbufs_of = {}

    def load_x(ti):
        bl = []
        for s in range(2):
            bi = sub_ctr[0] % NXS
            sub_ctr[0] += 1
            bl.append(bi)
            row = ti * 2 + s
            S.add("sp", lambda e, bi=bi, row=row: e.dma_start(out=xs[bi], in_=x_t[row]),
                  writes=[f"xs{bi}"], tag=f"xs{bi}")
        xbufs_of[ti] = bl

    GTOT = NT1 * 9

    def load_wb(G):
        wi_ = G % 3
        g = G % 9
        S.add("sp", lambda e, wi_=wi_, g=g: e.dma_start(out=wb[wi_], in_=w_in_bf[g]),
              reads=[f"w_in_bf{g}"], writes=[f"wb{wi_}"], tag=f"wb{wi_}")

    def stepA(ti, part="all"):
        par = ti % 2
        xbufs = xbufs_of[ti]
        for s in range(2 if part in ("all", "norm") else 0):
            bi = xbufs[s]
            S.add("act", lambda e, bi=bi, s=s, par=par: e.activation(out=hn[s], in_=xs[bi], func=AF.Square,
                                                                    accum_out=ssq[par][:, s:s + 1]),
                  reads=[f"xs{bi}"], writes=[f"ssq{par}", f"hn{s}"])
        if part in ("all", "norm"):
            S.add("act", lambda e, par=par: e.activation(out=std[par], in_=ssq[par], func=AF.Sqrt, scale=1.0 / D, bias=EPS),
                  reads=[f"ssq{par}"], writes=[f"std{par}"])
            S.add("dve", lambda e, par=par: e.reciprocal(out=rstd[par], in_=std[par]),
                  reads=[f"std{par}"], writes=[f"rstd{par}"])
            for s in range(2):
                bi = xbufs[s]
                S.add("dve", lambda e, bi=bi, s=s, par=par: e.tensor_scalar(
                    out=hn[s], in0=xs[bi], scalar1=rstd[par][:, s:s + 1], scalar2=None, op0=ALU.mult),
                    reads=[f"xs{bi}", f"rstd{par}"], writes=[f"hn{s}"])
        for s in range(2 if part in ("all", "T") else 0):
            hb = s
            tb = s
            tp = banks[tb][:].bitcast(BF16).rearrange("p (a b) -> p a b", a=8)
            for kc in range(8):
                S.add("pe", lambda e, kc=kc, hb=hb, tp=tp: e.transpose(out=tp[:, kc, :], in_=hn[hb][:, kc * 128:(kc + 1) * 128],
                                                                       identity=ident),
                      reads=[f"hn{hb}", "ident"], writes=[f"bank{tb}"])
            S.add("dve", lambda e, tp=tp, par=par, s=s: e.tensor_tensor(out=hT[par][:, :, s * 128:(s + 1) * 128], in0=tp, in1=g1bc,
                                                                        op=ALU.mult),
                  reads=[f"bank{tb}", "g1bc"], writes=[f"hT{par}"])

    def stepB(ti, groups, hook=None):
        par = ti % 2
        seq_start = (ti % TPS1 == 0)
        if seq_start and 0 in groups:
            S.add("dve", lambda e: e.memset(up[:, :, 0:16], 0.0), writes=[f"up{j}" for j in range(4)])
            S.add("dve", lambda e: e.memset(ul[:, :, 0:3], 0.0), writes=[f"ul{c}" for c in range(8)])
        for g in groups:
            G = ti * 9 + g
            wi_ = G % 3
            for j in range(4):
                oc = 4 * g + j
                ps, psn = next_slot()
                for kc in range(8):
                    S.add("pe", lambda e, ps=ps, wi_=wi_, kc=kc, j=j, par=par: e.matmul(
                        ps, lhsT=wb[wi_][:, kc, j * 128:(j + 1) * 128], rhs=hT[par][:, kc, :], start=(kc == 0), stop=(kc == 7)),
                        reads=[f"wb{wi_}", f"hT{par}"], writes=[psn])
                if oc < 4:
                    S.add("act", lambda e, ps=ps, oc=oc: e.activation(out=up[:, oc, 16:16 + T1], in_=ps, func=AF.Copy),
                          reads=[psn], writes=[f"up{oc}"])
                elif oc < 12:
                    c = oc - 4
                    S.add("act", lambda e, ps=ps, c=c: e.activation(out=ul[:, c, 3:3 + T1], in_=ps, func=AF.Copy),
                          reads=[psn], writes=[f"ul{c}"])
                elif oc < 20:
                    c = oc - 12
                    S.add("act", lambda e, ps=ps, c=c, par=par: e.activation(out=gg2[par][:, c, :], in_=ps, func=AF.Gelu_apprx_tanh),
                          reads=[psn], writes=[f"gg{par}_{c}"])
                elif oc < 28:
                    c = oc - 20
                    S.add("act", lambda e, ps=ps, c=c, par=par: e.activation(out=tha2[par][:, c, :], in_=ps, func=AF.Tanh, scale=0.5),
                          reads=[psn], writes=[f"tha{par}_{c}"])
                else:
                    c = oc - 28
                    S.add("act", lambda e, ps=ps, c=c, par=par: e.activation(out=thb2[par][:, c, :], in_=ps, func=AF.Tanh, scale=0.5),
                          reads=[psn], writes=[f"thb{par}_{c}"])
                if hook is not None:
                    hook(oc)
            if G + 3 < GTOT:
                load_wb(G + 3)

    def stepC(ti, part="all"):
        seq_start = (ti % TPS1 == 0)
        for j in range(4 if part in ("all", "dve") else 0):
            w = 2 << j
            U = up[:, j, :]
            cur = U
            curn = f"up{j}"
            d = 1
            tmps = [(s1, "s1"), (s2, "s2")]
            k = 0
            while d < w:
                dst, dstn = tmps[k % 2]
                lo = 2 * d - 1
                S.add("dve", lambda e, dst=dst, cur=cur, lo=lo, d=d: e.tensor_tensor(
                    out=dst[:, lo:UPW], in0=cur[:, lo:UPW], in1=cur[:, lo - d:UPW - d], op=ALU.add),
                    reads=[curn], writes=[dstn])
                cur, curn = dst, dstn
                d *= 2
                k += 1
            S.add("dve", lambda e, cur=cur, U=U, j=j, w=w: e.scalar_tensor_tensor(
                out=z[:, j, :], in0=cur[:, 16:UPW], scalar=1.0 / w, in1=U[:, 16:UPW], op0=ALU.mult, op1=ALU.subtract),
                reads=[curn, f"up{j}"], writes=[f"z{j}"])
            if seq_start:
                n = w - 1
                S.add("dve", lambda e, cur=cur, n=n: e.tensor_tensor(out=ptmp[:, 0:n], in0=cur[:, 16:16 + n], in1=invc[:, 0:n], op=ALU.mult),
                      reads=[curn, "invc"], writes=["ptmp"])
                S.add("dve", lambda e, U=U, n=n, j=j: e.tensor_tensor(out=z[:, j, 0:n], in0=ptmp[:, 0:n], in1=U[:, 16:16 + n], op=ALU.subtract),
                      reads=["ptmp", f"up{j}"], writes=[f"z{j}"])
            S.add("dve", lambda e, U=U: e.tensor_copy(out=U[:, 0:16], in_=U[:, T1:T1 + 16]),
                  reads=[f"up{j}"], writes=[f"up{j}"])
        for j in range(4 if part in ("all", "pe") else 0):
            ps, psn = next_slot()
            S.add("pe", lambda e, ps=ps, j=j: e.matmul(ps, lhsT=pl_w[:, j, :], rhs=z[:, j, :], start=True, stop=True),
                  reads=["pl_w", f"z{j}"], writes=[psn])
            S.add("act", lambda e, ps=ps, j=j: e.activation(out=pa[:, j, :], in_=ps, func=AF.Copy, scale=psT[:, j:j + 1]),
                  reads=[psn, "psT"], writes=[f"pa{j}"])

    def stepD(ti, between=None):
        slots = {}

        def s1(c):
            cb = c % 2
            ps, psn = next_slot()
            for k in range(4):
                S.add("pe", lambda e, ps=ps, k=k, c=c: e.matmul(ps, lhsT=dg[:, k, c, :], rhs=ul[:, c, k:k + T1],
                                                                start=(k == 0), stop=(k == 3)),
                      reads=["dg", f"ul{c}"], writes=[psn])
            S.add("act", lambda e, ps=ps, c=c, cb=cb: e.activation(out=xc[cb], in_=ps, func=AF.Identity, bias=cbT[:, c:c + 1]),
                  reads=[psn, "cbT"], writes=[f"xc{cb}"])
            S.add("dve", lambda e, c=c: e.tensor_copy(out=ul[:, c, 0:3], in_=ul[:, c, T1:T1 + 3]),
                  reads=[f"ul{c}"], writes=[f"ul{c}"])

        def s2(c):
            cb = c % 2
            psr, psrn = next_slot()
            S.add("pe", lambda e, psr=psr, c=c, cb=cb: e.matmul(psr, lhsT=wr_bd[:, c, :], rhs=xc[cb], start=True, stop=True),
                  reads=["wr_bd", f"xc{cb}"], writes=[psrn])
            psi, psin = next_slot()
            S.add("pe", lambda e, psi=psi, c=c, cb=cb: e.matmul(psi, lhsT=wi_bd[:, c, :], rhs=xc[cb], start=True, stop=True),
                  reads=["wi_bd", f"xc{cb}"], writes=[psin])
            S.add("act", lambda e, psr=psr, c=c, cb=cb: e.activation(out=thr[cb], in_=psr, func=AF.Tanh, scale=0.5, bias=hbr[:, c:c + 1]),
                  reads=[psrn, "hbr"], writes=[f"thr{cb}"])
            S.add("act", lambda e, psi=psi, c=c, cb=cb: e.activation(out=thi[cb], in_=psi, func=AF.Tanh, scale=0.5, bias=hbi[:, c:c + 1]),
                  reads=[psin, "hbi"], writes=[f"thi{cb}"])
            S.add("act", lambda e, c=c, cb=cb: e.activation(out=a_t[:, c, :], in_=thr[cb], func=AF.Exp,
                                                            scale=chalf[:, c:c + 1], bias=chalf[:, c:c + 1]),
                  reads=[f"thr{cb}", "chalf"], writes=[f"a{c}"])
            S.add("dve", lambda e, c=c, cb=cb: e.scalar_tensor_tensor(out=t_t[:, c, :], in0=thi[cb], scalar=1.0, in1=xc[cb],
                                                                      op0=ALU.add, op1=ALU.mult),
                  reads=[f"thi{cb}", f"xc{cb}"], writes=[f"t{c}"])

        s1(0)
        for c in range(8):
            if c + 1 < 8:
                s1(c + 1)
            if between is not None:
                between(c)
            s2(c)

    def stepE(ti, chunks=range(8)):
        par = ti % 2
        if ti % TPS1 == 0 and 0 in chunks:
            S.add("dve", lambda e: e.memset(hst, 0.0), writes=["hst"])
        for c in chunks:
            cb = c % 2
            S.add("act", lambda e, c=c, cb=cb: e.activation(out=mult[cb], in_=a_t[:, c, :], func=AF.Square),
                  reads=[f"a{c}"], writes=[f"mult{cb}"])
            S.add("act", lambda e, cb=cb: e.activation(out=mult[cb], in_=mult[cb], func=AF.Sqrt, scale=-0.25, bias=0.25),
                  reads=[f"mult{cb}"], writes=[f"mult{cb}"])
            S.add("dve", lambda e, c=c, cb=cb: e.tensor_tensor(out=t_t[:, c, :], in0=t_t[:, c, :], in1=mult[cb], op=ALU.mult),
                  reads=[f"t{c}", f"mult{cb}"], writes=[f"t{c}"])
            S.add("dve", lambda e, c=c, cb=cb: e.tensor_tensor_scan(out=hs[cb], data0=a_t[:, c, :], data1=t_t[:, c, :],
                                                                    initial=hst[:, c:c + 1], op0=ALU.mult, op1=ALU.add),
                  reads=[f"a{c}", f"t{c}", "hst"], writes=[f"hs{cb}"])
            S.add("dve", lambda e, c=c, cb=cb: e.tensor_copy(out=hst[:, c:c + 1], in_=hs[cb][:, T1 - 1:T1]),
                  reads=[f"hs{cb}"], writes=["hst"])
            S.add("dve", lambda e, c=c, cb=cb, par=par: e.tensor_tensor(out=hg[:, c, :], in0=hs[cb], in1=gg2[par][:, c, :], op=ALU.mult),
                  reads=[f"hs{cb}", f"gg{par}_{c}"], writes=[f"hg{c}"])

    def stepF(ti):
        par = ti % 2
        for oc in range(8):
            ps, psn = next_slot()
            for kc in range(4):
                S.add("pe", lambda e, ps=ps, kc=kc, oc=oc: e.matmul(ps, lhsT=pa_w[:, kc, oc * 128:(oc + 1) * 128], rhs=pa[:, kc, :],
                                                                    start=(kc == 0), stop=(kc == 3)),
                      reads=["pa_w", f"pa{kc}"], writes=[psn])
            S.add("dve", lambda e, ps=ps, oc=oc, par=par: e.scalar_tensor_tensor(out=m1[:, oc, :], in0=tha2[par][:, oc, :], scalar=1.0, in1=ps,
                                                                        op0=ALU.add, op1=ALU.mult),
                  reads=[psn, f"tha{par}_{oc}"], writes=[f"m1_{oc}"])
        for oc in range(8):
            ps, psn = next_slot()
            for kc in range(8):
                S.add("pe", lambda e, ps=ps, kc=kc, oc=oc: e.matmul(ps, lhsT=pb_w[:, kc, oc * 128:(oc + 1) * 128], rhs=hg[:, kc, :],
                                                                    start=(kc == 0), stop=(kc == 7)),
                      reads=["pb_w", f"hg{kc}"], writes=[psn])
            S.add("dve", lambda e, ps=ps, oc=oc, par=par: e.scalar_tensor_tensor(out=m2[:, oc, :], in0=thb2[par][:, oc, :], scalar=1.0, in1=ps,
                                                                        op0=ALU.add, op1=ALU.mult),
                  reads=[psn, f"thb{par}_{oc}"], writes=[f"m2_{oc}"])
            S.add("dve", lambda e, oc=oc: e.tensor_tensor(out=m1[:, oc, :], in0=m1[:, oc, :], in1=m2[:, oc, :], op=ALU.add),
                  reads=[f"m1_{oc}", f"m2_{oc}"], writes=[f"m1_{oc}"])

    def G_pieces(ti):
        xbufs = xbufs_of[ti]
        state = {}
        pieces = []
        for s in range(2):
            for half in range(2):
                for part in range(2):
                    def piece(s=s, half=half, part=part):
                        bi = xbufs[s]
                        row = ti * 2 + s
                        if part == 0:
                            state[(s, half)] = next_big()
                        ps, psn = state[(s, half)]
                        for kc in range(part * 4, part * 4 + 4):
                            S.add("pe", lambda e, ps=ps, kc=kc: e.matmul(
                                ps, lhsT=m1[:, kc, s * 128:(s + 1) * 128], rhs=wo_w[:, kc, half * 512:(half + 1) * 512],
                                start=(kc == 0), stop=(kc == 7)),
                                reads=["wo_w", f"m1_{kc}"], writes=[psn])
                        if part == 1:
                            S.add("dve", lambda e, ps=ps, bi=bi: e.scalar_tensor_tensor(
                                out=xs[bi][:, half * 512:(half + 1) * 512], in0=ps, scalar=0.5, in1=xs[bi][:, half * 512:(half + 1) * 512],
                                op0=ALU.mult, op1=ALU.add),
                                reads=[psn, f"xs{bi}"], writes=[f"xs{bi}"])
                            if half == 1:
                                dst = out_t[row] if stop_after_phase1 else x1_t[row]
                                S.add("sp", lambda e, bi=bi, dst=dst: e.dma_start(out=dst, in_=xs[bi]),
                                      reads=[f"xs{bi}"], writes=[f"x1o{bi}"], tag=f"xo{bi}")
                    pieces.append(piece)
        return pieces

    load_x(0)
    if NT1 > 1:
        load_x(1)
    for G in range(min(3, GTOT)):
        load_wb(G)
    stepA(0)
    stepB(0, list(range(9)))
    stepC(0)
    stepD(0)
    if NT1 > 1:
        stepA(1, "norm")
    for ti in range(NT1):
        nxt = ti + 1 < NT1
        if nxt:
            stepA(ti + 1, "T")
        if ti % 2 == 1 and cast_state[0] < NE:
            cast_expert(cast_state[0], [f"hT{(ti + 1) % 2}"])
            cast_state[0] += 1
        if ti + 2 < NT1:
            load_x(ti + 2)
        if nxt:
            def hook(oc, ti=ti):
                if oc % 2 == 1 and oc // 2 < 8:
                    stepE(ti, [oc // 2])
            stepE(ti, [])
            stepB(ti + 1, list(range(9)), hook=hook)
        else:
            stepE(ti)
        stepF(ti)
        if ti + 2 < NT1:
            stepA(ti + 2, "norm")
        pcs = G_pieces(ti)
        if nxt:
            stepC(ti + 1, "dve")
            stepD(ti + 1, between=lambda c: pcs[c]())
            stepC(ti + 1, "pe")
        else:
            for p_ in pcs:
                p_()

    while cast_state[0] < NE:
        cast_expert(cast_state[0], [])
        cast_state[0] += 1
    if stop_after_phase1:
        S.add("sp", None, reads=[f"x1o{b}" for b in range(NXS)])
    elif sparse:
        build_sparse_phase2(locals())
    else:
        S.barrier()
        A.off = A1_start
        x1t = [A.alloc([4, D], F32) for _ in range(2)]
        junk2 = A.alloc([D], BF16)
        hn2 = [A.alloc([D], BF16) for _ in range(2)]
        ssq2 = A.alloc([4], F32)
        std2 = A.alloc([4], F32)
        rstd2 = A.alloc([4], F32)
        h2T = A.alloc([8, T2], BF16)
        lg = A.alloc([4, 20], F32)
        gmax = A.alloc([4], F32)
        gsh = A.alloc([4, 4], F32)
        gsum = A.alloc([4], F32)
        ptop = A.alloc([4], F32)
        gmask = A.alloc([4, 4], F32)
        elm = A.alloc([4, 16], F32)
        mx8 = A.alloc([4, 8], F32)
        dd = A.alloc([4], F32)
        w1 = A.alloc([4], F32)
        w2 = A.alloc([4], F32)
        eq1 = A.alloc([4, 16], F32)
        eq2 = A.alloc([4, 16], F32)
        comb = A.alloc([4, 16], F32)
        wgb = [A.alloc([8, 512], BF16) for _ in range(2)]
        wub = [A.alloc([8, 512], BF16) for _ in range(2)]
        wdb = [A.alloc([4, D], BF16) for _ in range(2)]
        sg = [A.alloc([T2], BF16) for _ in range(2)]
        he = [A.alloc([4, T2], BF16) for _ in range(2)]
        ssq3 = A.alloc([4], F32)
        std3 = A.alloc([4], F32)
        rstd3 = A.alloc([4], F32)

        gu_banks = [2, 3, 4, 5]
        gu_ctr = [0]

        def next_gu():
            b = gu_banks[gu_ctr[0] % 4]
            gu_ctr[0] += 1
            return banks[b][:, :], f"bank{b}"

        dn_ctr = [0]

        def next_dn():
            b = (6, 7)[dn_ctr[0] % 2]
            dn_ctr[0] += 1
            return banks[b][:, :], f"bank{b}"

        ectr = [0]
        for ti in range(NT2):
            xb = ti % 2
            X = x1t[xb]
            S.add("sp", lambda e, X=X, ti=ti: e.dma_start(out=X, in_=x1s[ti * T2:(ti + 1) * T2, :].rearrange("(s p) d -> p s d", p=128)),
                  reads=["x1s"], writes=[f"x1t{xb}"], tag=f"x1t{xb}")
            for s in range(4):
                S.add("act", lambda e, X=X, s=s: e.activation(out=junk2, in_=X[:, s, :], func=AF.Square, accum_out=ssq2[:, s:s + 1]),
                      reads=[f"x1t{xb}"], writes=["ssq2"])
            S.add("act", lambda e: e.activation(out=std2, in_=ssq2, func=AF.Sqrt, scale=1.0 / D, bias=EPS),
                  reads=["ssq2"], writes=["std2"])
            S.add("dve", lambda e: e.reciprocal(out=rstd2, in_=std2), reads=["std2"], writes=["rstd2"])
            for s in range(4):
                hb = s % 2
                S.add("dve", lambda e, X=X, s=s, hb=hb: e.tensor_scalar(out=hn2[hb], in0=X[:, s, :], scalar1=rstd2[:, s:s + 1],
                                                                        scalar2=None, op0=ALU.mult),
                      reads=[f"x1t{xb}", "rstd2"], writes=[f"hn2_{hb}"])
                tp = banks[0][:].bitcast(BF16).rearrange("p (a b) -> p a b", a=8)
                for kc in range(8):
                    S.add("pe", lambda e, kc=kc, hb=hb, tp=tp: e.transpose(out=tp[:, kc, :], in_=hn2[hb][:, kc * 128:(kc + 1) * 128],
                                                                           identity=ident),
                          reads=[f"hn2_{hb}", "ident"], writes=["bank0"])
                S.add("dve", lambda e, tp=tp, s=s: e.tensor_tensor(out=h2T[:, :, s * 128:(s + 1) * 128], in0=tp, in1=g2bc, op=ALU.mult),
                      reads=["bank0", "g2bc"], writes=["h2T"])
            lgp = banks[1][:, 0:80].rearrange("p (s n) -> p s n", s=4)
            for s in range(4):
                for kc in range(8):
                    S.add("pe", lambda e, s=s, kc=kc: e.matmul(lgp[:, s, :], lhsT=h2T[:, kc, s * 128:(s + 1) * 128], rhs=rw[:, kc, :],
                                                               start=(kc == 0), stop=False),
                          reads=["h2T", "rw_a", "rw_b"], writes=["bank1"])
                S.add("pe", lambda e, s=s: e.matmul(lgp[:, s, :], lhsT=ones_bf[0:1, :], rhs=rb_bf[0:1, 0:20], start=False, stop=True),
                      reads=["ones_bf", "rb_bf"], writes=["bank1"])
            S.add("dve", lambda e: e.tensor_copy(out=lg, in_=lgp), reads=["bank1"], writes=["lg"])
            GL = lg[:, :, 0:4]
            EL = lg[:, :, 4:20]
            S.add("dve", lambda e: e.tensor_reduce(out=gmax, in_=GL, axis=AX.X, op=ALU.max), reads=["lg"], writes=["gmax"])
            gmax_bc = gmax.unsqueeze(2).to_broadcast([128, 4, 4])
            S.add("dve", lambda e: e.tensor_tensor(out=gsh, in0=GL, in1=gmax_bc, op=ALU.subtract), reads=["lg", "gmax"], writes=["gsh"])
            S.add("dve", lambda e: e.tensor_tensor(out=gmask, in0=GL, in1=gmax_bc, op=ALU.is_equal), reads=["lg", "gmax"], writes=["gmask"])
            S.add("act", lambda e: e.activation(out=gsh, in_=gsh, func=AF.Exp), reads=["gsh"], writes=["gsh"])
            S.add("dve", lambda e: e.tensor_reduce(out=gsum, in_=gsh, axis=AX.X, op=ALU.add), reads=["gsh"], writes=["gsum"])
            S.add("dve", lambda e: e.reciprocal(out=ptop, in_=gsum), reads=["gsum"], writes=["ptop"])
            S.add("dve", lambda e: e.tensor_scalar(out=gmask, in0=gmask, scalar1=-1.0, scalar2=1e30, op0=ALU.add, op1=ALU.mult),
                  reads=["gmask"], writes=["gmask"])
            for s in range(4):
                S.add("dve", lambda e, s=s: e.tensor_tensor(
                    out=elm[:, s, :].rearrange("p (g k) -> p g k", g=4), in0=EL[:, s, :].rearrange("p (g k) -> p g k", g=4),
                    in1=gmask[:, s, :].unsqueeze(2).to_broadcast([128, 4, 4]), op=ALU.add),
                    reads=["lg", "gmask"], writes=["elm"])
            for s in range(4):
                S.add("dve", lambda e, s=s: e.max(out=mx8[:, s, :], in_=elm[:, s, :]), reads=["elm"], writes=["mx8"])
            S.add("dve", lambda e: e.tensor_tensor(out=dd, in0=mx8[:, :, 1], in1=mx8[:, :, 0], op=ALU.subtract), reads=["mx8"], writes=["dd"])
            S.add("act", lambda e: e.activation(out=dd, in_=dd, func=AF.Exp), reads=["dd"], writes=["dd"])
            S.add("dve", lambda e: e.tensor_scalar(out=dd, in0=dd, scalar1=1.0, scalar2=None, op0=ALU.add), reads=["dd"], writes=["dd"])
            S.add("dve", lambda e: e.reciprocal(out=dd, in_=dd), reads=["dd"], writes=["dd"])
            S.add("dve", lambda e: e.tensor_tensor(out=w1, in0=ptop, in1=dd, op=ALU.mult), reads=["ptop", "dd"], writes=["w1"])
            S.add("dve", lambda e: e.tensor_tensor(out=w2, in0=ptop, in1=w1, op=ALU.subtract), reads=["ptop", "w1"], writes=["w2"])
            S.add("dve", lambda e: e.tensor_tensor(out=eq1, in0=elm, in1=mx8[:, :, 0:1].to_broadcast([128, 4, 16]), op=ALU.is_equal),
                  reads=["elm", "mx8"], writes=["eq1"])
            S.add("dve", lambda e: e.tensor_tensor(out=eq2, in0=elm, in1=mx8[:, :, 1:2].to_broadcast([128, 4, 16]), op=ALU.is_equal),
                  reads=["elm", "mx8"], writes=["eq2"])
            S.add("dve", lambda e: e.tensor_tensor(out=eq1, in0=eq1, in1=w1.unsqueeze(2).to_broadcast([128, 4, 16]), op=ALU.mult),
                  reads=["eq1", "w1"], writes=["eq1"])
            S.add("dve", lambda e: e.tensor_tensor(out=eq2, in0=eq2, in1=w2.unsqueeze(2).to_broadcast([128, 4, 16]), op=ALU.mult),
                  reads=["eq2", "w2"], writes=["eq2"])
            S.add("dve", lambda e: e.tensor_tensor(out=comb, in0=eq1, in1=eq2, op=ALU.add), reads=["eq1", "eq2"], writes=["comb"])
            for ex in range(NE):
                eb = ectr[0] % 2
                ectr[0] += 1
                S.add("sp", lambda e, eb=eb, ex=ex: e.dma_start(out=wgb[eb], in_=wg_bf[ex]), reads=["wg_bf"], writes=[f"wgb{eb}"], tag=f"wgb{eb}")
                S.add("sp", lambda e, eb=eb, ex=ex: e.dma_start(out=wub[eb], in_=wu_bf[ex]), reads=["wu_bf"], writes=[f"wub{eb}"], tag=f"wub{eb}")
                S.add("sp", lambda e, eb=eb, ex=ex: e.dma_start(out=wdb[eb], in_=wd_bf[ex]), reads=["wd_bf"], writes=[f"wdb{eb}"], tag=f"wdb{eb}")
                for hc in range(4):
                    pg, pgn = next_gu()
                    pu, pun = next_gu()
                    for kc in range(8):
                        S.add("pe", lambda e, pg=pg, kc=kc, hc=hc, eb=eb: e.matmul(pg, lhsT=wgb[eb][:, kc, hc * 128:(hc + 1) * 128], rhs=h2T[:, kc, :],
                                                                                  start=(kc == 0), stop=(kc == 7)),
                              reads=[f"wgb{eb}", "h2T"], writes=[pgn])
                    for kc in range(8):
                        S.add("pe", lambda e, pu=pu, kc=kc, hc=hc, eb=eb: e.matmul(pu, lhsT=wub[eb][:, kc, hc * 128:(hc + 1) * 128], rhs=h2T[:, kc, :],
                                                                                  start=(kc == 0), stop=(kc == 7)),
                              reads=[f"wub{eb}", "h2T"], writes=[pun])
                    sb = hc % 2
                    S.add("act", lambda e, pg=pg, sb=sb: e.activation(out=sg[sb], in_=pg, func=AF.Silu), reads=[pgn], writes=[f"sg{sb}"])
                    S.add("dve", lambda e, pu=pu, sb=sb, eb=eb, hc=hc: e.tensor_tensor(out=he[eb][:, hc, :], in0=sg[sb], in1=pu, op=ALU.mult),
                          reads=[pun, f"sg{sb}"], writes=[f"he{eb}_{hc}"])
                for s in range(4):
                    for half in range(2):
                        pd, pdn = next_dn()
                        for hc in range(4):
                            S.add("pe", lambda e, pd=pd, hc=hc, s=s, half=half, eb=eb: e.matmul(
                                pd, lhsT=he[eb][:, hc, s * 128:(s + 1) * 128], rhs=wdb[eb][:, hc, half * 512:(half + 1) * 512],
                                start=(hc == 0), stop=(hc == 3)),
                                reads=[f"he{eb}_{hc}", f"wdb{eb}"], writes=[pdn])
                        S.add("dve", lambda e, pd=pd, X=X, s=s, half=half, ex=ex: e.scalar_tensor_tensor(
                            out=X[:, s, half * 512:(half + 1) * 512], in0=pd, scalar=comb[:, s, ex:ex + 1],
                            in1=X[:, s, half * 512:(half + 1) * 512], op0=ALU.mult, op1=ALU.add),
                            reads=[pdn, "comb", f"x1t{xb}"], writes=[f"x1t{xb}"])
            for s in range(4):
                S.add("act", lambda e, X=X, s=s: e.activation(out=junk2, in_=X[:, s, :], func=AF.Square, accum_out=ssq3[:, s:s + 1]),
                      reads=[f"x1t{xb}"], writes=["ssq3"])
            S.add("act", lambda e: e.activation(out=std3, in_=ssq3, func=AF.Sqrt, scale=1.0 / D, bias=EPS),
                  reads=["ssq3"], writes=["std3"])
            S.add("dve", lambda e: e.reciprocal(out=rstd3, in_=std3), reads=["std3"], writes=["rstd3"])
            for s in range(4):
                S.add("dve", lambda e, X=X, s=s: e.scalar_tensor_tensor(out=X[:, s, :], in0=X[:, s, :], scalar=rstd3[:, s:s + 1], in1=gfbc,
                                                                       op0=ALU.mult, op1=ALU.mult),
                      reads=[f"x1t{xb}", "rstd3", "gfbc"], writes=[f"x1t{xb}"])
            S.add("sp", lambda e, X=X, ti=ti: e.dma_start(out=out[ti * T2:(ti + 1) * T2, :].rearrange("(s p) d -> p s d", p=128), in_=X),
                  reads=[f"x1t{xb}"], writes=[f"out{xb}"], tag=f"out{xb}")
        S.add("sp", None, reads=[f"out{b}" for b in range(NB3)])

    sem_es = ExitStack()
    es.enter_context(sem_es)
    S.emit(lambda name: sem_es.enter_context(nc.semaphore(name)))
    es.close()
    if os.environ.get("KDEBUG"):
        print("ops", S.nops, "waits", S.nwaits, "arena phase1 end", A1_end)
    return nc


_INPUT_ORDER = ["x", "norm1_g", "w_in", "pool_w", "pool_scale", "conv_w", "conv_b", "rg_w_r", "rg_b_r", "rg_w_i", "rg_b_i",
                "rg_lambda", "proj_a", "proj_b", "w_out", "norm2_g", "router_group_w", "router_group_b", "router_expert_w",
                "router_expert_b", "exp_w_gate", "exp_w_up", "exp_w_down", "norm_f_g"]


def make_in_maps(inputs, n_cores, n_seq, seq_len):
    shared = {}
    for k in _INPUT_ORDER:
        if k == "x":
            continue
        a = np.asarray(inputs[k], dtype=np.float32)
        if k != "norm_f_g":
            a = a[0]
        shared[k] = np.ascontiguousarray(a)
    xfull = np.asarray(inputs["x"], dtype=np.float32)
    in_maps = []
    for c in range(n_cores):
        m = dict(shared)
        m["x"] = np.ascontiguousarray(xfull[c * n_seq:(c + 1) * n_seq].reshape(n_seq * seq_len, D))
        in_maps.append(m)
    return in_maps


def kernel(**inputs):
    x = np.asarray(inputs["x"])
    B, SEQ, _ = x.shape
    n_seq = B // NCORES
    nc = build_nc(n_seq=n_seq, seq_len=SEQ)
    in_maps = make_in_maps(inputs, NCORES, n_seq, SEQ)
    res = run_bass_kernel_spmd(nc, in_maps, core_ids=list(range(NCORES)))
    outs = [np.asarray(r["out"]).reshape(n_seq, SEQ, D) for r in res.results]
    return np.concatenate(outs, axis=0).astype(np.float32)
```
